# Optimizing a Trainium2 kernel written in Bass

```python
import math
import jax, jax.numpy as jnp
from jax import lax
import numpy as np

D_MODEL = 1024
BATCH = 4
SEQ = 4096
DEPTH = 4

HEAD_DIM = 64
MIX_WIDTH = D_MODEL
SB_WIDTH = MIX_WIDTH // 2
SB_HEADS = SB_WIDTH // HEAD_DIM
SSM_WIDTH = MIX_WIDTH - SB_WIDTH
SSM_GROUP = 16
SSM_GROUPS = SSM_WIDTH // SSM_GROUP
SSM_STATE = 64
DSA_HEADS = MIX_WIDTH // HEAD_DIM
DSA_KV_HEADS = 4
DSA_REP = DSA_HEADS // DSA_KV_HEADS
DSA_WIDTH = DSA_HEADS * HEAD_DIM
IDX_HEADS = 8
IDX_DIM = 32
TOPK_MAX = 256
D_FF = 4 * D_MODEL
Q_BLOCK = 128
N_EVEN = (DEPTH + 1) // 2
N_ODD = DEPTH // 2
ALPHA = (2 * DEPTH) ** 0.25
BETA = (8 * DEPTH) ** -0.25
LN_EPS = 1e-5
SB_IN = 3 * SB_WIDTH + SSM_WIDTH
DSA_SIZES = (DSA_WIDTH, DSA_KV_HEADS * HEAD_DIM, DSA_KV_HEADS * HEAD_DIM, IDX_HEADS * IDX_DIM, IDX_DIM, IDX_HEADS)
DSA_IN = sum(DSA_SIZES)
DSA_SPLITS = tuple(int(v) for v in np.cumsum(DSA_SIZES)[:-1])

kernel_name = "hybrid_stickbreak_s5_dsa_deepnorm"


def layer_norm(x, g, b):
    xf = x.astype(jnp.float32)
    mu = jnp.mean(xf, axis=-1, keepdims=True)
    var = jnp.mean(jnp.square(xf - mu), axis=-1, keepdims=True)
    y = (xf - mu) * lax.rsqrt(var + LN_EPS) * g.astype(jnp.float32) + b.astype(jnp.float32)
    return y.astype(x.dtype)


def to_blocks(a, nb):
    return jnp.moveaxis(a.reshape((a.shape[0], nb, Q_BLOCK) + a.shape[2:]), 1, 0)


def stick_breaking_attention(q, k, v):
    Bn, S, H, dh = q.shape
    nb = S // Q_BLOCK
    spos = jnp.arange(S)
    scale = dh ** -0.5

    def block(args):
        q_b, bid = args
        tpos = bid * Q_BLOCK + jnp.arange(Q_BLOCK)
        z = jnp.einsum('bqhd,bshd->bhqs', q_b, k).astype(jnp.float32) * scale
        causal = (spos[None, :] < tpos[:, None])[None, None]
        log_beta = jax.nn.log_sigmoid(z)
        log_1mb = jnp.where(causal, jax.nn.log_sigmoid(-z), 0.0)
        csum = jnp.cumsum(log_1mb, axis=-1)
        log_w = log_beta + csum[..., -1:] - csum
        w = jnp.where(causal, jnp.exp(log_w), 0.0).astype(v.dtype)
        return jnp.einsum('bhqs,bshd->bqhd', w, v)

    out = lax.map(block, (to_blocks(q, nb), jnp.arange(nb)))
    return jnp.moveaxis(out, 0, 1).reshape(Bn, S, H * dh)


def _complex_scan_op(e_i, e_j):
    air, aii, bir, bii = e_i
    ajr, aji, bjr, bji = e_j
    ar = ajr * air - aji * aii
    ai = ajr * aii + aji * air
    br = ajr * bir - aji * bii + bjr
    bi = ajr * bii + aji * bir + bji
    return (ar, ai, br, bi)


def s5_ssm(u, log_dt, lam_re, lam_im, b_re, b_im, c_re, c_im, d):
    f32 = jnp.float32
    S = u.shape[1]
    dt = jnp.exp(log_dt.astype(f32))[:, None]
    lr = lam_re.astype(f32)
    li = lam_im.astype(f32)
    mag = jnp.exp(lr * dt)
    abar_r = mag * jnp.cos(li * dt)
    abar_i = mag * jnp.sin(li * dt)
    den = lr * lr + li * li
    nr = abar_r - 1.0
    coef_r = ((nr * lr + abar_i * li) / den)[..., None]
    coef_i = ((abar_i * lr - nr * li) / den)[..., None]
    br = b_re.astype(f32)
    bi = b_im.astype(f32)
    bbar_r = coef_r * br - coef_i * bi
    bbar_i = coef_r * bi + coef_i * br
    uf = u.astype(f32)
    bu_r = jnp.einsum('bsgc,gnc->bsgn', uf, bbar_r)
    bu_i = jnp.einsum('bsgc,gnc->bsgn', uf, bbar_i)
    a_r = jnp.broadcast_to(abar_r, (1, S) + abar_r.shape)
    a_i = jnp.broadcast_to(abar_i, (1, S) + abar_i.shape)
    _, _, x_r, x_i = lax.associative_scan(_complex_scan_op, (a_r, a_i, bu_r, bu_i), axis=1)
    y = (jnp.einsum('gcn,bsgn->bsgc', c_re.astype(f32), x_r)
         - jnp.einsum('gcn,bsgn->bsgc', c_im.astype(f32), x_i)
         + d.astype(f32) * uf)
    return y.astype(u.dtype)


def mixer_sb_ssm(x, w_in, log_dt, lam_re, lam_im, b_re, b_im, c_re, c_im, d, w_glu, b_glu, w_out):
    Bn, S, _ = x.shape
    proj = x @ w_in
    q, k, v, u = jnp.split(proj, [SB_WIDTH, 2 * SB_WIDTH, 3 * SB_WIDTH], axis=-1)
    hs = (Bn, S, SB_HEADS, HEAD_DIM)
    a_out = stick_breaking_attention(q.reshape(hs), k.reshape(hs), v.reshape(hs))
    y = s5_ssm(u.reshape(Bn, S, SSM_GROUPS, SSM_GROUP), log_dt, lam_re, lam_im,
               b_re, b_im, c_re, c_im, d).reshape(Bn, S, SSM_WIDTH)
    y = jax.nn.gelu(y)
    y = y * jax.nn.sigmoid(y @ w_glu + b_glu)
    return jnp.concatenate([a_out, y], axis=-1) @ w_out


def alibi_slopes(n_heads):
    return jnp.asarray(2.0 ** (-8.0 * np.arange(1, n_heads + 1) / n_heads), dtype=jnp.float32)


def mixer_dsa(x, w_in, w_out):
    Bn, S, _ = x.shape
    proj = x @ w_in
    q, k, v, qi, ki, wi = jnp.split(proj, DSA_SPLITS, axis=-1)
    q = q.reshape(Bn, S, DSA_KV_HEADS, DSA_REP, HEAD_DIM)
    k = k.reshape(Bn, S, DSA_KV_HEADS, HEAD_DIM)
    v = v.reshape(Bn, S, DSA_KV_HEADS, HEAD_DIM)
    qi = qi.reshape(Bn, S, IDX_HEADS, IDX_DIM)
    topk = min(TOPK_MAX, S // 4)
    nb = S // Q_BLOCK
    spos = jnp.arange(S)
    slopes = alibi_slopes(DSA_HEADS).reshape(DSA_KV_HEADS, DSA_REP)
    idx_scale = (IDX_DIM ** -0.5) * (IDX_HEADS ** -0.5)

    def block(args):
        q_b, qi_b, wi_b, bid = args
        tpos = bid * Q_BLOCK + jnp.arange(Q_BLOCK)
        rel = jax.nn.relu(jnp.einsum('bqhd,bsd->bqsh', qi_b, ki).astype(jnp.float32))
        score = jnp.einsum('bqsh,bqh->bqs', rel, wi_b.astype(jnp.float32)) * idx_scale
        causal = (spos[None, :] <= tpos[:, None])[None]
        score = jnp.where(causal, score, -jnp.inf)
        _, idx = lax.top_k(score, topk)
        k_sel = jax.vmap(lambda kb, ib: kb[ib])(k, idx)
        v_sel = jax.vmap(lambda vb, ib: vb[ib])(v, idx)
        logits = jnp.einsum('bqgrd,bqkgd->bqgrk', q_b, k_sel).astype(jnp.float32) * (HEAD_DIM ** -0.5)
        dist = (tpos[None, :, None] - idx).astype(jnp.float32)
        logits = logits - slopes[None, None, :, :, None] * dist[:, :, None, None, :]
        valid = (idx <= tpos[None, :, None])[:, :, None, None, :]
        logits = jnp.where(valid, logits, -jnp.inf)
        p = jax.nn.softmax(logits, axis=-1).astype(v.dtype)
        o = jnp.einsum('bqgrk,bqkgd->bqgrd', p, v_sel)
        return o.reshape(Bn, Q_BLOCK, DSA_WIDTH)

    out = lax.map(block, (to_blocks(q, nb), to_blocks(qi, nb), to_blocks(wi, nb), jnp.arange(nb)))
    out = jnp.moveaxis(out, 0, 1).reshape(Bn, S, DSA_WIDTH)
    return out @ w_out


def sq_relu_mlp(x, w1, w2):
    return jnp.square(jax.nn.relu(x @ w1)) @ w2


def setup_inputs(seed: int = 0) -> dict:
    key = jax.random.key(seed)
    ks = jax.random.split(key, 32)
    f32 = jnp.float32

    def nrm(k, shape, scale):
        return jax.random.normal(k, shape, f32) * scale

    ws = D_MODEL ** -0.5
    x = nrm(ks[0], (BATCH, SEQ, D_MODEL), 1.0)
    sb_ssm_w_in = jnp.concatenate([
        nrm(ks[1], (N_EVEN, D_MODEL, 2 * SB_WIDTH), ws),
        nrm(ks[2], (N_EVEN, D_MODEL, SB_WIDTH), ws * BETA),
        nrm(ks[3], (N_EVEN, D_MODEL, SSM_WIDTH), ws)], axis=-1)
    ssm_log_dt = jax.random.uniform(ks[4], (N_EVEN, SSM_GROUPS), f32,
                                    minval=math.log(1e-3), maxval=math.log(1e-1))
    n = jnp.arange(SSM_STATE, dtype=f32)
    ssm_lam_re = -0.5 + nrm(ks[5], (N_EVEN, SSM_GROUPS, SSM_STATE), 0.01)
    ssm_lam_im = math.pi * n + nrm(ks[6], (N_EVEN, SSM_GROUPS, SSM_STATE), 0.01)
    bsc = (2 * SSM_GROUP) ** -0.5
    ssm_b_re = nrm(ks[7], (N_EVEN, SSM_GROUPS, SSM_STATE, SSM_GROUP), bsc)
    ssm_b_im = nrm(ks[8], (N_EVEN, SSM_GROUPS, SSM_STATE, SSM_GROUP), bsc)
    csc = (2 * SSM_STATE) ** -0.5
    ssm_c_re = nrm(ks[9], (N_EVEN, SSM_GROUPS, SSM_GROUP, SSM_STATE), csc)
    ssm_c_im = nrm(ks[10], (N_EVEN, SSM_GROUPS, SSM_GROUP, SSM_STATE), csc)
    ssm_d = nrm(ks[11], (N_EVEN, SSM_GROUPS, SSM_GROUP), 1.0)
    ssm_w_glu = nrm(ks[12], (N_EVEN, SSM_WIDTH, SSM_WIDTH), SSM_WIDTH ** -0.5)
    ssm_b_glu = nrm(ks[13], (N_EVEN, SSM_WIDTH), 0.02)
    sb_ssm_w_out = nrm(ks[14], (N_EVEN, MIX_WIDTH, D_MODEL), (MIX_WIDTH ** -0.5) * BETA)
    kvw = DSA_KV_HEADS * HEAD_DIM
    dsa_w_in = jnp.concatenate([
        nrm(ks[15], (N_ODD, D_MODEL, DSA_WIDTH + kvw), ws),
        nrm(ks[16], (N_ODD, D_MODEL, kvw), ws * BETA),
        nrm(ks[17], (N_ODD, D_MODEL, IDX_HEADS * IDX_DIM + IDX_DIM + IDX_HEADS), ws)], axis=-1)
    dsa_w_out = nrm(ks[18], (N_ODD, DSA_WIDTH, D_MODEL), (DSA_WIDTH ** -0.5) * BETA)
    ln_mix_g = 1.0 + nrm(ks[19], (DEPTH, D_MODEL), 0.02)
    ln_mix_b = nrm(ks[20], (DEPTH, D_MODEL), 0.02)
    ln_ffn_g = 1.0 + nrm(ks[21], (DEPTH, D_MODEL), 0.02)
    ln_ffn_b = nrm(ks[22], (DEPTH, D_MODEL), 0.02)
    mlp_w1 = nrm(ks[23], (DEPTH, D_MODEL, D_FF), ws * BETA)
    mlp_w2 = nrm(ks[24], (DEPTH, D_FF, D_MODEL), (D_FF ** -0.5) * BETA)
    return {"x": x, "sb_ssm_w_in": sb_ssm_w_in, "ssm_log_dt": ssm_log_dt,
            "ssm_lam_re": ssm_lam_re, "ssm_lam_im": ssm_lam_im,
            "ssm_b_re": ssm_b_re, "ssm_b_im": ssm_b_im,
            "ssm_c_re": ssm_c_re, "ssm_c_im": ssm_c_im, "ssm_d": ssm_d,
            "ssm_w_glu": ssm_w_glu, "ssm_b_glu": ssm_b_glu, "sb_ssm_w_out": sb_ssm_w_out,
            "dsa_w_in": dsa_w_in, "dsa_w_out": dsa_w_out,
            "ln_mix_g": ln_mix_g, "ln_mix_b": ln_mix_b,
            "ln_ffn_g": ln_ffn_g, "ln_ffn_b": ln_ffn_b,
            "mlp_w1": mlp_w1, "mlp_w2": mlp_w2}


def reference(x, sb_ssm_w_in, ssm_log_dt, ssm_lam_re, ssm_lam_im, ssm_b_re, ssm_b_im,
              ssm_c_re, ssm_c_im, ssm_d, ssm_w_glu, ssm_b_glu, sb_ssm_w_out,
              dsa_w_in, dsa_w_out, ln_mix_g, ln_mix_b, ln_ffn_g, ln_ffn_b, mlp_w1, mlp_w2):
    for i in range(DEPTH):
        j = i // 2
        if i % 2 == 0:
            h = mixer_sb_ssm(x, sb_ssm_w_in[j], ssm_log_dt[j], ssm_lam_re[j], ssm_lam_im[j],
                             ssm_b_re[j], ssm_b_im[j], ssm_c_re[j], ssm_c_im[j], ssm_d[j],
                             ssm_w_glu[j], ssm_b_glu[j], sb_ssm_w_out[j])
        else:
            h = mixer_dsa(x, dsa_w_in[j], dsa_w_out[j])
        x = layer_norm(ALPHA * x + h, ln_mix_g[i], ln_mix_b[i])
        x = layer_norm(ALPHA * x + sq_relu_mlp(x, mlp_w1[i], mlp_w2[i]), ln_ffn_g[i], ln_ffn_b[i])
    return x
```

```python
import math
from contextlib import ExitStack, contextmanager

import numpy as np
import concourse.bass as bass
import concourse.mybir as mybir
from concourse.bass_utils import run_bass_kernel_spmd

F32 = mybir.dt.float32
BF16 = mybir.dt.bfloat16
I32 = mybir.dt.int32
AF = mybir.ActivationFunctionType
ALU = mybir.AluOpType
AX = mybir.AxisListType

D = 1024
T = 4096
NT = T // 128
DEPTH = 4
DFF = 4096
ALPHA = (2 * DEPTH) ** 0.25
LN_EPS = 1e-5
NCORES = 4
TOPK = 256
BIG = 1.0e30
NBISECT = 22
MASKBIG = 131072.0


class Buf:
    __slots__ = ("name", "w", "r")

    def __init__(self, name="b"):
        self.name = name
        self.w = None
        self.r = {}


class Ring:
    def __init__(self, items):
        self.items = items
        self.i = 0

    def next(self):
        it = self.items[self.i % len(self.items)]
        self.i += 1
        return it


class Prog:
    ENG = ("pe", "act", "dve", "pool", "sp")
    NDMA = 40

    def __init__(self, nc, es):
        self.nc = nc
        self.es = es
        self.eng = {"pe": nc.tensor, "act": nc.scalar, "dve": nc.vector,
                    "pool": nc.gpsimd, "sp": nc.sync}
        self.es_root = es
        self.epoch = 0
        self.key = {e: e + "#0" for e in self.ENG}
        self.sem = {e: es.enter_context(nc.semaphore("s_" + e)) for e in self.ENG}
        self.cnt = {e: 0 for e in self.ENG}
        self.dsem = [es.enter_context(nc.semaphore("d%d" % i)) for i in range(self.NDMA)]
        self.dcnt = [0] * self.NDMA
        self.dnext = 0
        self.seen = {e: {} for e in self.ENG}
        self.nins = 0
        self.uid = 0
        self._fregs = {}

    def freg(self, v):
        v = float(v)
        if v not in self._fregs:
            self._fregs[v] = self.nc.gpsimd.to_reg(v)
        return self._fregs[v]

    def sb(self, shape, dt, name=None):
        self.uid += 1
        return self.es.enter_context(self.nc.sbuf_tensor("%s_%d" % (name or "t", self.uid), list(shape), dt))

    def ps(self, shape, dt=F32, name=None):
        self.uid += 1
        return self.es.enter_context(self.nc.psum_tensor("%s_%d" % (name or "p", self.uid), list(shape), dt))

    def sbring(self, n, shape, dt, name=None):
        return Ring([(self.sb(shape, dt, name), Buf()) for _ in range(n)])

    def psring(self, n, shape, dt=F32, name=None):
        return Ring([(self.ps(shape, dt, name), Buf()) for _ in range(n)])

    @contextmanager
    def phase(self):
        old = self.es
        with ExitStack() as st:
            self.es = st
            yield
            self.barrier()
            if max(self.cnt.values()) > 9000:
                self.new_epoch()
        self.es = old

    def new_epoch(self):
        self.epoch += 1
        for e in ("pe", "act", "dve", "pool"):
            if self.cnt[e] == 0:
                continue
            self.sem[e] = self.es_root.enter_context(self.nc.semaphore("s_%s_%d" % (e, self.epoch)))
            self.cnt[e] = 0
            self.key[e] = "%s#%d" % (e, self.epoch)

    def _deps(self, reads, writes):
        evs = []
        for b in reads:
            if b.w is not None:
                evs.append(b.w)
        for b in writes:
            if b.w is not None:
                evs.append(b.w)
            evs.extend(b.r.values())
        return evs

    def _wait(self, e, evs):
        best = {}
        for (k, s, v) in evs:
            if self.seen[e].get(k, 0) >= v:
                continue
            if k not in best or best[k][1] < v:
                best[k] = (s, v)
        for k, (s, v) in best.items():
            self.seen[e][k] = v
            self.eng[e].wait_ge(s, v)

    def _record(self, ev, reads, writes):
        k = ev[0]
        for b in reads:
            b.r[k] = ev
        for b in writes:
            b.w = ev
            b.r = {}

    def op(self, e, fn, reads=(), writes=()):
        evs = self._deps(reads, writes)
        if e == "pe":
            evs = [x for x in evs if not x[0].startswith("pe#")]
        self._wait(e, evs)
        ins = fn(self.eng[e])
        self.cnt[e] += 1
        ins.then_inc(self.sem[e], 1)
        ev = (self.key[e], self.sem[e], self.cnt[e])
        self._record(ev, reads, writes)
        self.nins += 1
        return ev

    def dma(self, qe, out, in_, reads=(), writes=(), **kw):
        evs = self._deps(reads, writes)
        i = self.dnext
        self.dnext = (self.dnext + 1) % self.NDMA
        key = "d%d" % i
        if self.dcnt[i] > 0:
            evs = list(evs) + [(key, self.dsem[i], self.dcnt[i])]
        self._wait(qe, evs)
        self.dcnt[i] += 16
        ins = self.eng[qe].dma_start(out=out, in_=in_, **kw)
        ins.then_inc(self.dsem[i], 16)
        ev = (key, self.dsem[i], self.dcnt[i])
        self._record(ev, reads, writes)
        self.nins += 1
        return ev

    def barrier(self):
        evs = [(self.key[e], self.sem[e], self.cnt[e]) for e in self.ENG if self.cnt[e] > 0]
        evs += [("d%d" % i, self.dsem[i], self.dcnt[i]) for i in range(self.NDMA) if self.dcnt[i] > 0]
        for e in self.ENG:
            self._wait(e, evs)


def make_identity(P, dt=F32):
    ident = P.sb([128, 128], F32, "ident")
    b = Buf()
    P.op("pool", lambda e: e.memset(ident[:], 1.0), writes=[b])
    P.op("pool", lambda e: e.affine_select(out=ident[:], in_=ident[:], pattern=[[-1, 128]],
                                           compare_op=ALU.is_equal, fill=P.freg(0.0), base=0,
                                           channel_multiplier=1), reads=[b], writes=[b])
    if dt == F32:
        return ident, b
    idb = P.sb([128, 128], dt, "identb")
    bb = Buf()
    P.op("dve", lambda e: e.tensor_copy(out=idb[:], in_=ident[:]), reads=[b], writes=[bb])
    return idb, bb


def cast_engine_op(P, k, out, in_, reads, writes):
    e = ("dve", "pool", "act")[k % 3]
    if e == "act":
        return P.op("act", lambda g: g.copy(out=out, in_=in_), reads=reads, writes=writes)
    return P.op(e, lambda g: g.tensor_copy(out=out, in_=in_), reads=reads, writes=writes)


def phase_cast(P, jobs):
    CH = 2048
    with P.phase():
        st32 = P.sbring(3, [128, CH], F32, "st32")
        st16 = P.sbring(3, [128, CH], BF16, "st16")
        k = 0
        for (src, cmap) in jobs:
            R, C = src.shape
            for r0 in range(0, R, 128):
                for c0 in range(0, C, CH):
                    cn = min(CH, C - c0)
                    a, ba = st32.next()
                    b, bb = st16.next()
                    P.dma("sp", a[:, :cn], src[r0:r0 + 128, c0:c0 + cn], writes=[ba])
                    cast_engine_op(P, k, b[:, :cn], a[:, :cn], [ba], [bb])
                    k += 1
                    for (s0, n, dst, d0) in cmap:
                        lo = max(s0, c0)
                        hi = min(s0 + n, c0 + cn)
                        if lo >= hi:
                            continue
                        P.dma("act" if (k % 2) else "sp", dst[r0:r0 + 128, d0 + lo - s0:d0 + hi - s0],
                              b[:, lo - c0:hi - c0], reads=[bb])


def transpose_store(P, ident, bident, src, bsrc, a, stage, bstage, ptr):
    for half in range(2):
        pt, bpt = ptr.next()
        for kk in range(4):
            k = half * 4 + kk
            P.op("pe", lambda e, k=k, kk=kk, pt=pt: e.transpose(out=pt[:, kk * 128:(kk + 1) * 128],
                                                            in_=src[:, k * 128:(k + 1) * 128],
                                                            identity=ident[:]),
                 reads=[bsrc, bident], writes=[bpt])
        o = stage[:, half * 4:(half + 1) * 4, a * 128:(a + 1) * 128]
        i = pt[:].rearrange("p (k t) -> p k t", k=4)
        if half == 0:
            P.op("act", lambda e, o=o, i=i: e.copy(out=o, in_=i), reads=[bpt], writes=[bstage])
        else:
            P.op("dve", lambda e, o=o, i=i: e.tensor_copy(out=o, in_=i), reads=[bpt], writes=[bstage])


def phase_transpose(P, x_tm, xT):
    with P.phase():
        ident, bident = make_identity(P)
        xin = P.sbring(3, [128, D], F32, "xin")
        stg = P.sbring(2, [128, 8, 512], BF16, "stg")
        ptr = P.psring(4, [128, 512], F32, "ptr")
        for c in range(T // 512):
            stage, bstage = stg.next()
            for a in range(4):
                xt, bx = xin.next()
                t0 = c * 512 + a * 128
                P.dma("sp", xt[:], x_tm[t0:t0 + 128, :], writes=[bx])
                transpose_store(P, ident, bident, xt, bx, a, stage, bstage, ptr)
            P.dma("sp", xT.rearrange("(k p) t -> p k t", p=128)[:, :, c * 512:(c + 1) * 512],
                  stage[:], reads=[bstage])


def phase_inproj(P, xT, W, ncols, fm_tiles, tm_specs):
    with P.phase():
        Wsb = P.sb([128, 8, ncols], BF16, "Wsb")
        bW = Buf()
        for k in range(8):
            P.dma("sp" if k % 2 else "act", Wsb[:, k, :], W[k * 128:(k + 1) * 128, 0:ncols], writes=[bW])
        xr = P.sbring(2, [128, 8, 512], BF16, "xTc")
        pr = P.psring(4, [128, 512], F32, "pp")
        orr = P.sbring(4, [128, 512], BF16, "ofm")
        otm = {}
        for (c0, n, dst, dt) in tm_specs:
            otm[c0] = P.sbring(3, [128, n], dt, "otm")
        xTv = xT.rearrange("(k p) t -> p k t", p=128)
        cnt = 0
        for c in range(T // 512):
            xc, bxc = xr.next()
            P.dma("sp", xc[:], xTv[:, :, c * 512:(c + 1) * 512], writes=[bxc])
            for ft in fm_tiles:
                c0, dst, r0 = ft[0], ft[1], ft[2]
                scl = ft[3] if len(ft) > 3 else 1.0
                pt, bpt = pr.next()
                for k in range(8):
                    P.op("pe", lambda e, k=k, pt=pt, c0=c0, xc=xc: e.matmul(
                        out=pt[:], lhsT=Wsb[:, k, c0:c0 + 128], rhs=xc[:, k, :],
                        start=(k == 0), stop=(k == 7)), reads=[bW, bxc], writes=[bpt])
                ot, bot = orr.next()
                if cnt % 2 == 0:
                    P.op("act", lambda e, ot=ot, pt=pt, scl=scl: e.mul(out=ot[:], in_=pt[:], mul=scl), reads=[bpt], writes=[bot])
                else:
                    P.op("dve", lambda e, ot=ot, pt=pt, scl=scl: e.tensor_scalar(out=ot[:], in0=pt[:], scalar1=scl, scalar2=None,
                                                                              op0=ALU.mult), reads=[bpt], writes=[bot])
                cnt += 1
                P.dma("sp", dst[r0:r0 + 128, c * 512:(c + 1) * 512], ot[:], reads=[bot])
            for (c0, n, dst, dt) in tm_specs:
                for a in range(4):
                    pt, bpt = pr.next()
                    for k in range(8):
                        P.op("pe", lambda e, k=k, pt=pt, c0=c0, n=n, a=a, xc=xc: e.matmul(
                            out=pt[:, :n], lhsT=xc[:, k, a * 128:(a + 1) * 128], rhs=Wsb[:, k, c0:c0 + n],
                            start=(k == 0), stop=(k == 7)), reads=[bW, bxc], writes=[bpt])
                    ot, bot = otm[c0].next()
                    if cnt % 2 == 0:
                        P.op("act", lambda e, ot=ot, pt=pt, n=n: e.copy(out=ot[:], in_=pt[:, :n]), reads=[bpt], writes=[bot])
                    else:
                        P.op("dve", lambda e, ot=ot, pt=pt, n=n: e.tensor_copy(out=ot[:], in_=pt[:, :n]), reads=[bpt], writes=[bot])
                    cnt += 1
                    t0 = c * 512 + a * 128
                    P.dma("sp", dst[t0:t0 + 128, :], ot[:], reads=[bot])


def ln_tile(P, r, br, gbc, bbc, bgb, scr):
    st, bst = scr["st"].next()
    mv, bmv = scr["mv"].next()
    for h in range(2):
        P.op("dve", lambda e, h=h: e.bn_stats(out=st[:, h * 6:(h + 1) * 6], in_=r[:, h * 512:(h + 1) * 512]),
             reads=[br], writes=[bst])
    P.op("dve", lambda e: e.bn_aggr(out=mv[:, 0:2], in_=st[:, 0:12]), reads=[bst], writes=[bmv])
    P.op("dve", lambda e: e.tensor_scalar(out=mv[:, 2:3], in0=mv[:, 1:2], scalar1=LN_EPS, scalar2=None,
                                          op0=ALU.add), reads=[bmv], writes=[bmv])
    P.op("act", lambda e: e.activation(out=mv[:, 3:4], in_=mv[:, 2:3], func=AF.Sqrt), reads=[bmv], writes=[bmv])
    P.op("dve", lambda e: e.reciprocal(out=mv[:, 4:5], in_=mv[:, 3:4]), reads=[bmv], writes=[bmv])
    P.op("dve", lambda e: e.tensor_scalar(out=r[:], in0=r[:], scalar1=mv[:, 0:1], scalar2=mv[:, 4:5],
                                          op0=ALU.subtract, op1=ALU.mult), reads=[br, bmv], writes=[br])
    P.op("pool", lambda e: e.tensor_tensor(out=r[:], in0=r[:], in1=gbc[:], op=ALU.mult), reads=[br, bgb], writes=[br])
    P.op("dve", lambda e: e.tensor_tensor(out=r[:], in0=r[:], in1=bbc[:], op=ALU.add), reads=[br, bgb], writes=[br])


def load_gb(P, g_ap, b_ap):
    gbc = P.sb([128, D], F32, "gbc")
    bbc = P.sb([128, D], F32, "bbc")
    bgb = Buf()
    P.dma("sp", gbc[:], g_ap.to_broadcast([128, D]), writes=[bgb])
    P.dma("sp", bbc[:], b_ap.to_broadcast([128, D]), writes=[bgb])
    return gbc, bbc, bgb


def ln_scratch(P):
    return {"st": P.sbring(3, [128, 12], F32, "lnst"), "mv": P.sbring(3, [128, 8], F32, "lnmv")}


def phase_outproj_ln(P, catT, Wout, x_tm, g_ap, b_ap, x1_tm, x1T):
    with P.phase():
        ident, bident = make_identity(P)
        Wsb = P.sb([128, 8, D], BF16, "Wo")
        bW = Buf()
        for k in range(8):
            P.dma("sp" if k % 2 else "act", Wsb[:, k, :], Wout[k * 128:(k + 1) * 128, :], writes=[bW])
        gbc, bbc, bgb = load_gb(P, g_ap, b_ap)
        scr = ln_scratch(P)
        cr = P.sbring(2, [128, 8, 512], BF16, "catc")
        xr = P.sbring(3, [128, D], F32, "xres")
        rr = P.sbring(3, [128, D], F32, "rr")
        stg = P.sbring(2, [128, 8, 512], BF16, "stg")
        pr = P.psring(4, [128, 512], F32, "pp")
        ptr = P.psring(4, [128, 512], F32, "ptr")
        cv = catT.rearrange("(k p) t -> p k t", p=128)
        for c in range(T // 512):
            cc, bcc = cr.next()
            P.dma("sp", cc[:], cv[:, :, c * 512:(c + 1) * 512], writes=[bcc])
            stage, bstage = stg.next()
            for a in range(4):
                t0 = c * 512 + a * 128
                xt, bx = xr.next()
                P.dma("sp", xt[:], x_tm[t0:t0 + 128, :], writes=[bx])
                r, br = rr.next()
                for oc in range(2):
                    pt, bpt = pr.next()
                    for k in range(8):
                        P.op("pe", lambda e, k=k, pt=pt, oc=oc, a=a, cc=cc: e.matmul(
                            out=pt[:], lhsT=cc[:, k, a * 128:(a + 1) * 128], rhs=Wsb[:, k, oc * 512:(oc + 1) * 512],
                            start=(k == 0), stop=(k == 7)), reads=[bW, bcc], writes=[bpt])
                    P.op("dve", lambda e, pt=pt, oc=oc, r=r, xt=xt: e.scalar_tensor_tensor(
                        out=r[:, oc * 512:(oc + 1) * 512], in0=xt[:, oc * 512:(oc + 1) * 512], scalar=ALPHA,
                        in1=pt[:], op0=ALU.mult, op1=ALU.add), reads=[bpt, bx], writes=[br])
                ln_tile(P, r, br, gbc, bbc, bgb, scr)
                P.dma("sp", x1_tm[t0:t0 + 128, :], r[:], reads=[br])
                transpose_store(P, ident, bident, r, br, a, stage, bstage, ptr)
            P.dma("sp", x1T.rearrange("(k p) t -> p k t", p=128)[:, :, c * 512:(c + 1) * 512],
                  stage[:], reads=[bstage])


def phase_mlp(P, x1T, x1_tm, W1, W2, g_ap, b_ap, x2_tm, x2T):
    with P.phase():
        ident, bident = make_identity(P)
        W1sb = P.sb([128, 8, DFF], BF16, "W1")
        bW1 = Buf()
        for k in range(8):
            P.dma("sp" if k % 2 else "act", W1sb[:, k, :], W1[k * 128:(k + 1) * 128, :], writes=[bW1])
        gbc, bbc, bgb = load_gb(P, g_ap, b_ap)
        scr = ln_scratch(P)
        xr = P.sbring(2, [128, 8, 512], BF16, "x1c")
        hT = P.sb([128, 32, 512], BF16, "hT")
        bh = [Buf() for _ in range(32)]
        sq = P.sbring(3, [128, 512], F32, "sq")
        w2r = P.sbring(4, [128, 512], BF16, "w2")
        xres = P.sbring(2, [128, D], F32, "xres")
        rr = [(P.sb([128, D], F32, "rr"), Buf()) for _ in range(4)]
        stg = P.sbring(1, [128, 8, 512], BF16, "stg")
        ph = P.psring(2, [128, 512], F32, "ph")
        pacc = [(P.ps([128, 512], F32, "acc"), Buf()) for _ in range(4)]
        ptr = P.psring(2, [128, 512], F32, "ptr")
        xv = x1T.rearrange("(k p) t -> p k t", p=128)
        for c in range(T // 512):
            xc, bxc = xr.next()
            P.dma("sp", xc[:], xv[:, :, c * 512:(c + 1) * 512], writes=[bxc])
            for f in range(32):
                pt, bpt = ph.next()
                for k in range(8):
                    P.op("pe", lambda e, k=k, pt=pt, f=f, xc=xc: e.matmul(
                        out=pt[:], lhsT=W1sb[:, k, f * 128:(f + 1) * 128], rhs=xc[:, k, :],
                        start=(k == 0), stop=(k == 7)), reads=[bW1, bxc], writes=[bpt])
                s, bs = sq.next()
                P.op("act", lambda e, s=s, pt=pt: e.activation(out=s[:], in_=pt[:], func=AF.Square),
                     reads=[bpt], writes=[bs])
                P.op("dve", lambda e, s=s, pt=pt, f=f: e.scalar_tensor_tensor(
                    out=hT[:, f, :], in0=pt[:], scalar=0.0, in1=s[:], op0=ALU.is_gt, op1=ALU.mult),
                    reads=[bpt, bs], writes=[bh[f]])
            for oc in range(2):
                for f in range(32):
                    w2, bw2 = w2r.next()
                    P.dma("sp" if f % 2 else "act", w2[:], W2[f * 128:(f + 1) * 128, oc * 512:(oc + 1) * 512], writes=[bw2])
                    for a in range(4):
                        P.op("pe", lambda e, a=a, f=f, w2=w2: e.matmul(
                            out=pacc[a][0][:], lhsT=hT[:, f, a * 128:(a + 1) * 128], rhs=w2[:],
                            start=(f == 0), stop=(f == 31)), reads=[bh[f], bw2], writes=[pacc[a][1]])
                for a in range(4):
                    t0 = c * 512 + a * 128
                    xt, bx = xres.next()
                    P.dma("sp", xt[:, :512], x1_tm[t0:t0 + 128, oc * 512:(oc + 1) * 512], writes=[bx])
                    r, br = rr[a]
                    P.op("dve", lambda e, a=a, oc=oc, r=r, xt=xt: e.scalar_tensor_tensor(
                        out=r[:, oc * 512:(oc + 1) * 512], in0=xt[:, :512], scalar=ALPHA,
                        in1=pacc[a][0][:], op0=ALU.mult, op1=ALU.add), reads=[pacc[a][1], bx], writes=[br])
            stage, bstage = stg.next()
            for a in range(4):
                t0 = c * 512 + a * 128
                r, br = rr[a]
                ln_tile(P, r, br, gbc, bbc, bgb, scr)
                P.dma("sp", x2_tm[t0:t0 + 128, :], r[:], reads=[br])
                if x2T is not None:
                    transpose_store(P, ident, bident, r, br, a, stage, bstage, ptr)
            if x2T is not None:
                P.dma("sp", x2T.rearrange("(k p) t -> p k t", p=128)[:, :, c * 512:(c + 1) * 512],
                      stage[:], reads=[bstage])


def phase_sb_attn(P, QK, V, CAT):
    with P.phase():
        qsb = P.sb([128, 4, T], BF16, "qsb")
        ksb = P.sb([128, 4, T], BF16, "ksb")
        vsb = P.sb([128, NT, 512], BF16, "vsb")
        bq, bk, bv = Buf(), Buf(), Buf()
        QKv = QK.rearrange("(o p) t -> p o t", p=128)
        for o in range(4):
            P.dma("sp", qsb[:, o, :], QKv[:, o, :], writes=[bq])
            P.dma("act", ksb[:, o, :], QKv[:, 4 + o, :], writes=[bk])
        Vv = V.rearrange("(j p) c -> p j c", p=128)
        for j0 in range(0, NT, 8):
            P.dma("sp", vsb[:, j0:j0 + 8, :], Vv[:, j0:j0 + 8, :], writes=[bv])
        U32 = P.sb([128, 128], F32, "U32")
        U = P.sb([128, 128], BF16, "U")
        ones = P.sb([128, 128], BF16, "ones")
        bc = Buf()
        P.op("pool", lambda e: e.memset(U32[:], 1.0), writes=[bc])
        P.op("pool", lambda e: e.affine_select(out=U32[:], in_=U32[:], pattern=[[1, 128]], compare_op=ALU.is_ge,
                                               fill=P.freg(0.0), base=0, channel_multiplier=-1), reads=[bc], writes=[bc])
        P.op("dve", lambda e: e.tensor_copy(out=U[:], in_=U32[:]), reads=[bc], writes=[bc])
        P.op("dve", lambda e: e.memset(ones[:], 1.0), writes=[bc])

        zr = P.psring(3, [128, 512], F32, "z")
        tr = P.psring(1, [128, 512], F32, "tq")
        Rr = P.psring(2, [128, 512], F32, "R")
        Or = P.psring(2, [128, 512], F32, "O")
        er = P.sbring(2, [128, 512], F32, "e")
        spr = P.sbring(6, [128, 512], F32, "sp")
        lr = P.sbring(3, [128, 512], BF16, "L")
        tmr = P.sbring(3, [128, 512], F32, "tmp")
        wr = P.sbring(3, [128, 512], BF16, "w")
        osr = P.sbring(2, [64, 512], BF16, "os")

        units = []
        for hp in range(4):
            for c in range(T // 512):
                grp = [(2 * hp + hh, Rr.next(), Or.next()) for hh in range(2)]
                jmax = 4 * c + 3
                for j in range(jmax, -1, -1):
                    for (h, Rb, Ob) in grp:
                        units.append(dict(h=h, c=c, j=j, jmax=jmax, R=Rb, O=Ob))

        def mask_op(u, tile, btile):
            base = u["c"] * 512 - u["j"] * 128
            P.op("pool", lambda e: e.affine_select(out=tile[:], in_=tile[:], pattern=[[1, 512]],
                                                   compare_op=ALU.is_gt, fill=P.freg(0.0), base=base,
                                                   channel_multiplier=-1), reads=[btile], writes=[btile])

        def st_a(u):
            h, c, j = u["h"], u["c"], u["j"]
            o, pb = h // 2, (h % 2) * 64
            z, bz = zr.next()
            P.op("pe", lambda e: e.matmul(out=z[:], lhsT=ksb[pb:pb + 64, o, j * 128:(j + 1) * 128],
                                          rhs=qsb[pb:pb + 64, o, c * 512:(c + 1) * 512], start=True, stop=True),
                 reads=[bq, bk], writes=[bz])
            u.update(z=z, bz=bz, diag=(j >= 4 * c))

        def st_b(u):
            z, bz = u["z"], u["bz"]
            ee, be = er.next()
            P.op("act", lambda e: e.activation(out=ee[:], in_=z[:], func=AF.Exp, scale=-0.125), reads=[bz], writes=[be])
            sp, bsp = spr.next()
            P.op("act", lambda e: e.activation(out=sp[:], in_=ee[:], func=AF.Ln, bias=1.0, scale=1.0), reads=[be], writes=[bsp])
            u.update(sp=sp, bsp=bsp)

        def st_c(u):
            z, bz, sp, bsp = u["z"], u["bz"], u["sp"], u["bsp"]
            L, bL = lr.next()
            P.op("dve", lambda e: e.scalar_tensor_tensor(out=L[:], in0=z[:], scalar=-0.125, in1=sp[:],
                                                         op0=ALU.mult, op1=ALU.subtract), reads=[bz, bsp], writes=[bL])
            u.update(L=L, bL=bL)
            if u["diag"]:
                mask_op(u, L, bL)

        def st_d(u):
            j = u["j"]
            L, bL = u["L"], u["bL"]
            R, bR = u["R"]
            tq, btq = tr.next()
            P.op("pe", lambda e: e.matmul(out=tq[:], lhsT=U[:], rhs=L[:], start=True, stop=True), reads=[bL, bc], writes=[btq])
            P.op("pe", lambda e: e.matmul(out=R[:], lhsT=ones[:], rhs=L[:], start=(j == u["jmax"]), stop=True),
                 reads=[bL, bc], writes=[bR])
            u.update(tq=tq, btq=btq)

        def st_e(u):
            tq, btq, sp, bsp = u["tq"], u["btq"], u["sp"], u["bsp"]
            R, bR = u["R"]
            tm, btm = tmr.next()
            P.op("dve", lambda e: e.scalar_tensor_tensor(out=tm[:], in0=tq[:], scalar=-1.0, in1=sp[:],
                                                         op0=ALU.mult, op1=ALU.subtract), reads=[btq, bsp], writes=[btm])
            P.op("dve", lambda e: e.tensor_tensor(out=tm[:], in0=tm[:], in1=R[:], op=ALU.add), reads=[btm, bR], writes=[btm])
            u.update(tm=tm, btm=btm)

        def st_f(u):
            tm, btm = u["tm"], u["btm"]
            w, bw = wr.next()
            P.op("act", lambda e: e.activation(out=w[:], in_=tm[:], func=AF.Exp), reads=[btm], writes=[bw])
            u.update(w=w, bw=bw)
            if u["diag"]:
                mask_op(u, w, bw)

        def st_g(u):
            h, c, j = u["h"], u["c"], u["j"]
            O, bO = u["O"]
            w, bw = u["w"], u["bw"]
            P.op("pe", lambda e: e.matmul(out=O[0:64, :], lhsT=vsb[:, j, h * 64:(h + 1) * 64], rhs=w[:],
                                          start=(j == u["jmax"]), stop=(j == 0)), reads=[bw, bv], writes=[bO])
            if j == 0:
                os_, bos = osr.next()
                P.op("act", lambda e: e.copy(out=os_[:], in_=O[0:64, :]), reads=[bO], writes=[bos])
                P.dma("sp", CAT[h * 64:(h + 1) * 64, c * 512:(c + 1) * 512], os_[:], reads=[bos])

        stages = [st_a, st_b, st_c, st_d, st_e, st_f, st_g]
        n = len(units)
        for i in range(n + len(stages) - 1):
            for k in range(len(stages) - 1, -1, -1):
                if 0 <= i - k < n:
                    stages[k](units[i - k])


LC = 256


def phase_ssm(P, UT, prm, Wg, CAT):
    (log_dt, lam_re, lam_im, b_re, b_im, c_re, c_im, dskip, bglu) = prm
    NCH = T // LC
    TW = LC + 1
    TWOPI = 2.0 * math.pi
    with P.phase():
        ident, bident = make_identity(P)
        SINT = P.sb([128, 16, TW], F32, "sint")
        COST = P.sb([128, 16, TW], F32, "cost")
        btab = Buf()
        BDr = P.sb([128, 16, 128], BF16, "BDr")
        BDi = P.sb([128, 16, 128], BF16, "BDi")
        CTr = P.sb([128, 16, 128], BF16, "CTr")
        CTi = P.sb([128, 16, 128], BF16, "CTi")
        bBD, bCT = Buf(), Buf()
        prm_t = P.sb([128, 16, 16], F32, "prm")
        bprm = Buf()
        dcol = P.sb([128, 4], F32, "dcol")
        bgcol = P.sb([128, 4], F32, "bgcol")
        bsm = Buf()
        usb = P.sb([128, 4, T], BF16, "usb")
        ygsb = P.sb([128, 4, T], BF16, "ygsb")
        bu_, byg = Buf(), [Buf() for _ in range(NCH * 4)]
        Wgsb = P.sb([128, 4, 512], BF16, "Wg")
        bWg = Buf()
        pbu = P.psring(4, [128, 512], F32, "pbu")
        py = P.psring(3, [128, 512], F32, "py")

        UTv = UT.rearrange("(q p) t -> p q t", p=128)
        for q in range(4):
            P.dma("sp" if q % 2 else "act", usb[:, q, :], UTv[:, q, :], writes=[bu_])
            P.dma("sp", Wgsb[:, q, :], Wg[q * 128:(q + 1) * 128, :], writes=[bWg])
        def load_T(src2d, R, dst, bdst):
            st = P.sb([128, 128], F32, "ldT")
            b = Buf()
            P.dma("sp", st[0:R, :], src2d, writes=[b])
            pt, bpt = pbu.next()
            P.op("pe", lambda e: e.transpose(out=pt[:, 0:R], in_=st[0:R, :], identity=ident[0:R, 0:R]),
                 reads=[b, bident], writes=[bpt])
            P.op("act", lambda e: e.copy(out=dst, in_=pt[:, 0:R]), reads=[bpt], writes=[bdst])
        load_T(dskip.rearrange("g c -> (g c)").rearrange("(q p) -> q p", p=128), 4, dcol[:], bsm)
        load_T(bglu.rearrange("(q p) -> q p", p=128), 4, bgcol[:], bsm)

        old_es = P.es
        with ExitStack() as st2:
            P.es = st2
            def col(k):
                return prm_t[:, :, k]
            load_T(lam_re.rearrange("(i two) n -> i (two n)", two=2), 16, col(0), bprm)
            load_T(lam_im.rearrange("(i two) n -> i (two n)", two=2), 16, col(1), bprm)
            ld2 = P.sb([16, 2], F32, "ld2")
            ldb = P.sb([16, 128], F32, "ldb")
            bld = Buf()
            P.dma("sp", ld2[:], log_dt.rearrange("(i two) -> i two", two=2), writes=[bld])
            for two in range(2):
                P.op("dve", lambda e, two=two: e.tensor_copy(out=ldb[:, 64 * two:64 * two + 64],
                                                            in_=ld2[:, two:two + 1].to_broadcast([16, 64])),
                     reads=[bld], writes=[bld])
            ptl, bptl = pbu.next()
            P.op("pe", lambda e: e.transpose(out=ptl[:, 0:16], in_=ldb[:], identity=ident[0:16, 0:16]),
                 reads=[bld, bident], writes=[bptl])
            P.op("act", lambda e: e.copy(out=col(2), in_=ptl[:, 0:16]), reads=[bptl], writes=[bprm])

            def vop(fn):
                P.op("dve", fn, reads=[bprm], writes=[bprm])

            def aop(fn):
                P.op("act", fn, reads=[bprm], writes=[bprm])
            aop(lambda e: e.activation(out=col(2), in_=col(2), func=AF.Exp))
            vop(lambda e: e.tensor_tensor(out=col(3), in0=col(0), in1=col(2), op=ALU.mult))
            aop(lambda e: e.activation(out=col(3), in_=col(3), func=AF.Exp))
            vop(lambda e: e.tensor_tensor(out=col(4), in0=col(1), in1=col(2), op=ALU.mult))
            vop(lambda e: e.tensor_scalar(out=col(5), in0=col(4), scalar1=1.0 / TWOPI, scalar2=None, op0=ALU.mult))
            itmp = P.sb([128, 16], I32, "itmp")
            vop(lambda e: e.tensor_copy(out=itmp[:], in_=col(5)))
            vop(lambda e: e.tensor_copy(out=col(13), in_=itmp[:]))
            vop(lambda e: e.tensor_tensor(out=col(6), in0=col(5), in1=col(13), op=ALU.subtract))
            iot = P.sb([128, TW], F32, "iot")
            P.op("pool", lambda e: e.iota(out=iot[:], pattern=[[1, TW]], base=0, channel_multiplier=0,
                                          allow_small_or_imprecise_dtypes=True), writes=[bprm])
            angr = P.sbring(2, [128, TW], F32, "ang")
            angi = P.sbring(2, [128, TW], I32, "angi")
            angc = P.sbring(2, [128, TW], F32, "angc")
            SC = TWOPI * (1.0 - 2e-6)
            for i in range(16):
                a, ba = angr.next()
                ai, bai = angi.next()
                ac, bac = angc.next()
                P.op("dve", lambda e, a=a, i=i: e.tensor_scalar(out=a[:], in0=iot[:], scalar1=prm_t[:, i, 6:7], scalar2=None,
                                                                op0=ALU.mult), reads=[bprm], writes=[ba])
                P.op("dve", lambda e, a=a, ai=ai: e.tensor_copy(out=ai[:], in_=a[:]), reads=[ba], writes=[bai])
                P.op("dve", lambda e, ac=ac, ai=ai: e.tensor_copy(out=ac[:], in_=ai[:]), reads=[bai], writes=[bac])
                P.op("dve", lambda e, a=a, ac=ac: e.tensor_tensor(out=a[:], in0=a[:], in1=ac[:], op=ALU.subtract),
                     reads=[ba, bac], writes=[ba])
                P.op("act", lambda e, a=a, i=i: e.activation(out=SINT[:, i, :], in_=a[:], func=AF.Sin, scale=SC),
                     reads=[ba], writes=[btab])
                P.op("dve", lambda e, a=a, ac=ac: e.tensor_scalar(out=ac[:], in0=a[:], scalar1=0.25, scalar2=0.5,
                                                                  op0=ALU.add, op1=ALU.is_gt), reads=[ba, bac], writes=[bac])
                P.op("dve", lambda e, a=a, ac=ac: e.scalar_tensor_tensor(out=a[:], in0=a[:], scalar=0.25, in1=ac[:],
                                                                         op0=ALU.add, op1=ALU.subtract),
                     reads=[ba, bac], writes=[ba])
                P.op("act", lambda e, a=a, i=i: e.activation(out=COST[:, i, :], in_=a[:], func=AF.Sin, scale=SC),
                     reads=[ba], writes=[btab])
            P.op("dve", lambda e: e.tensor_tensor(out=col(7), in0=col(3), in1=COST[:, :, 1], op=ALU.mult), reads=[bprm, btab], writes=[bprm])
            P.op("dve", lambda e: e.tensor_tensor(out=col(8), in0=col(3), in1=SINT[:, :, 1], op=ALU.mult), reads=[bprm, btab], writes=[bprm])
            vop(lambda e: e.tensor_tensor(out=col(9), in0=col(0), in1=col(0), op=ALU.mult))
            vop(lambda e: e.tensor_tensor(out=col(13), in0=col(1), in1=col(1), op=ALU.mult))
            vop(lambda e: e.tensor_tensor(out=col(9), in0=col(9), in1=col(13), op=ALU.add))
            vop(lambda e: e.reciprocal(out=col(9), in_=col(9)))
            vop(lambda e: e.tensor_scalar(out=col(10), in0=col(7), scalar1=-1.0, scalar2=None, op0=ALU.add))
            vop(lambda e: e.tensor_tensor(out=col(13), in0=col(10), in1=col(0), op=ALU.mult))
            vop(lambda e: e.tensor_tensor(out=col(14), in0=col(8), in1=col(1), op=ALU.mult))
            vop(lambda e: e.tensor_tensor(out=col(13), in0=col(13), in1=col(14), op=ALU.add))
            vop(lambda e: e.tensor_tensor(out=col(11), in0=col(13), in1=col(9), op=ALU.mult))
            vop(lambda e: e.tensor_tensor(out=col(13), in0=col(8), in1=col(0), op=ALU.mult))
            vop(lambda e: e.tensor_tensor(out=col(14), in0=col(10), in1=col(1), op=ALU.mult))
            vop(lambda e: e.tensor_tensor(out=col(13), in0=col(13), in1=col(14), op=ALU.subtract))
            vop(lambda e: e.tensor_tensor(out=col(12), in0=col(13), in1=col(9), op=ALU.mult))

            XBr = P.sb([128, 16, 128], F32, "XBr")
            XBi = P.sb([128, 16, 128], F32, "XBi")
            Bsr = P.sb([128, 16, 16], F32, "Bsr")
            Bsi = P.sb([128, 16, 16], F32, "Bsi")
            Cnr = P.sb([128, 4, 64], F32, "Cnr")
            Cni = P.sb([128, 4, 64], F32, "Cni")
            bX, bBs, bCn = Buf(), Buf(), Buf()
            P.op("dve", lambda e: e.memset(XBr[:], 0.0), writes=[bX])
            P.op("pool", lambda e: e.memset(XBi[:], 0.0), writes=[bX])
            P.op("dve", lambda e: e.memset(CTr[:], 0.0), writes=[bCT])
            P.op("pool", lambda e: e.memset(CTi[:], 0.0), writes=[bCT])
            P.dma("sp", Bsr[:], b_re.rearrange("g n c -> (g n) c").rearrange("(i p) c -> p i c", p=128), writes=[bBs])
            P.dma("act", Bsi[:], b_im.rearrange("g n c -> (g n) c").rearrange("(i p) c -> p i c", p=128), writes=[bBs])
            P.dma("sp", Cnr[:], c_re.rearrange("g c n -> (g c) n").rearrange("(q p) n -> p q n", p=128), writes=[bCn])
            P.dma("act", Cni[:], c_im.rearrange("g c n -> (g c) n").rearrange("(q p) n -> p q n", p=128), writes=[bCn])
            for (X, Bs) in ((XBr, Bsr), (XBi, Bsi)):
                X4 = X[:].rearrange("p (q r) c -> p q r c", r=4)
                B4 = Bs[:].rearrange("p (q r) c -> p q r c", r=4)
                for r in range(4):
                    for two in range(2):
                        c0 = 32 * r + 16 * two
                        o_ = X4[64 * two:64 * two + 64, :, r, c0:c0 + 16]
                        i_ = B4[64 * two:64 * two + 64, :, r, :]
                        P.op("dve", lambda e, o_=o_, i_=i_: e.tensor_copy(out=o_, in_=i_), reads=[bBs, bX], writes=[bX])
            tmpr = P.sbring(2, [128, 128], F32, "tmpx")
            bbr = P.sbring(2, [128, 128], F32, "bbx")
            for i in range(16):
                cr_, ci_ = prm_t[:, i, 11:12], prm_t[:, i, 12:13]
                for which in range(2):
                    tm, btm = tmpr.next()
                    bb, bbb = bbr.next()
                    if which == 0:
                        P.op("dve", lambda e, tm=tm, i=i, ci_=ci_: e.tensor_scalar(out=tm[:], in0=XBi[:, i, :], scalar1=ci_, scalar2=None, op0=ALU.mult),
                             reads=[bX, bprm], writes=[btm])
                        P.op("dve", lambda e, tm=tm, bb=bb, i=i, cr_=cr_: e.scalar_tensor_tensor(out=bb[:], in0=XBr[:, i, :], scalar=cr_, in1=tm[:],
                                                                                         op0=ALU.mult, op1=ALU.subtract),
                             reads=[bX, bprm, btm], writes=[bbb])
                    else:
                        P.op("dve", lambda e, tm=tm, i=i, ci_=ci_: e.tensor_scalar(out=tm[:], in0=XBr[:, i, :], scalar1=ci_, scalar2=None, op0=ALU.mult),
                             reads=[bX, bprm], writes=[btm])
                        P.op("dve", lambda e, tm=tm, bb=bb, i=i, cr_=cr_: e.scalar_tensor_tensor(out=bb[:], in0=XBi[:, i, :], scalar=cr_, in1=tm[:],
                                                                                         op0=ALU.mult, op1=ALU.add),
                             reads=[bX, bprm, btm], writes=[bbb])
                    pt, bpt = pbu.next()
                    P.op("pe", lambda e, pt=pt, bb=bb: e.transpose(out=pt[:, 0:128], in_=bb[:], identity=ident[:]),
                         reads=[bbb, bident], writes=[bpt])
                    dst = (BDr if which == 0 else BDi)[:, i, :]
                    P.op("act", lambda e, dst=dst, pt=pt: e.copy(out=dst, in_=pt[:, 0:128]), reads=[bpt], writes=[bBD])
            for which, (Cn, CT) in enumerate(((Cnr, CTr), (Cni, CTi))):
                sgn = 1.0 if which == 0 else -1.0
                for qp in range(4):
                    pt, bpt = pbu.next()
                    P.op("pe", lambda e, pt=pt, Cn=Cn, qp=qp: e.transpose(out=pt[0:64, 0:128], in_=Cn[:, qp, :], identity=ident[:]),
                         reads=[bCn, bident], writes=[bpt])
                    for r in range(4):
                        i = 4 * qp + r
                        for two in range(2):
                            c0 = 32 * r + 16 * two
                            P.op("act", lambda e, pt=pt, CT=CT, i=i, two=two, c0=c0, sgn=sgn: e.mul(
                                out=CT[64 * two:64 * two + 64, i, c0:c0 + 16], in_=pt[0:64, c0:c0 + 16], mul=sgn),
                                reads=[bpt, bCT], writes=[bCT])
            P.barrier()
        P.es = old_es

        R2 = lambda n, dt=F32, nm="r": P.sbring(n, [128, LC], dt, nm)
        bur_s, bui_s = R2(2, F32, "burs"), R2(2, F32, "buis")
        t1r, t2r, t3r, t4r = R2(2), R2(2), R2(2), R2(2)
        bpr, bpi = R2(2, F32, "bpr"), R2(2, F32, "bpi")
        xpr, xpi = R2(2, F32, "xpr"), R2(2, F32, "xpi")
        u1r, u2r, u3r, u4r = R2(2), R2(2), R2(2), R2(2)
        xrr, xir = R2(6, BF16, "xr"), R2(6, BF16, "xi")
        yr_, sqr_, p1r, p2r, sgr = R2(2), R2(2), R2(2), R2(2), R2(2)
        zsg = P.sbring(2, [128, LC], F32, "zsg")
        gor = P.sbring(3, [128, LC], BF16, "go")
        inits = [[(P.sb([128, 4], F32, "init"), Buf()) for _ in range(2)] for _ in range(16)]
        for i in range(16):
            P.op("dve", lambda e, i=i: e.memset(inits[i][0][0][:], 0.0), writes=[inits[i][0][1]])

        for k in range(NCH):
            tsl = slice(k * LC, (k + 1) * LC)
            for q in range(4):
                xs = []
                for r in range(4):
                    i = 4 * q + r
                    cosT, sinT = COST[:, i, 0:LC], SINT[:, i, 0:LC]
                    pr_, bpr_ = pbu.next()
                    pi_, bpi_ = pbu.next()
                    P.op("pe", lambda e, pr_=pr_, i=i: e.matmul(out=pr_[:, :LC], lhsT=BDr[:, i, :], rhs=usb[:, q, tsl], start=True, stop=True),
                         reads=[bBD, bu_], writes=[bpr_])
                    P.op("pe", lambda e, pi_=pi_, i=i: e.matmul(out=pi_[:, :LC], lhsT=BDi[:, i, :], rhs=usb[:, q, tsl], start=True, stop=True),
                         reads=[bBD, bu_], writes=[bpi_])
                    brs, bbrs = bur_s.next()
                    bis, bbis = bui_s.next()
                    P.op("act", lambda e, brs=brs, pr_=pr_: e.copy(out=brs[:], in_=pr_[:, :LC]), reads=[bpr_], writes=[bbrs])
                    P.op("act", lambda e, bis=bis, pi_=pi_: e.copy(out=bis[:], in_=pi_[:, :LC]), reads=[bpi_], writes=[bbis])
                    t1, b1 = t1r.next(); t2, b2 = t2r.next(); t3, b3 = t3r.next(); t4, b4 = t4r.next()
                    P.op("dve", lambda e, t1=t1, pr_=pr_, cosT=cosT: e.tensor_tensor(out=t1[:], in0=pr_[:, :LC], in1=cosT, op=ALU.mult), reads=[bpr_, btab], writes=[b1])
                    P.op("pool", lambda e, t2=t2, bis=bis, sinT=sinT: e.tensor_tensor(out=t2[:], in0=bis[:], in1=sinT, op=ALU.mult), reads=[bbis, btab], writes=[b2])
                    P.op("dve", lambda e, t3=t3, pi_=pi_, cosT=cosT: e.tensor_tensor(out=t3[:], in0=pi_[:, :LC], in1=cosT, op=ALU.mult), reads=[bpi_, btab], writes=[b3])
                    P.op("pool", lambda e, t4=t4, brs=brs, sinT=sinT: e.tensor_tensor(out=t4[:], in0=brs[:], in1=sinT, op=ALU.mult), reads=[bbrs, btab], writes=[b4])
                    br_, bbr_ = bpr.next(); bi_, bbi_ = bpi.next()
                    P.op("dve", lambda e, br_=br_, t1=t1, t2=t2: e.tensor_tensor(out=br_[:], in0=t1[:], in1=t2[:], op=ALU.add), reads=[b1, b2], writes=[bbr_])
                    P.op("pool", lambda e, bi_=bi_, t3=t3, t4=t4: e.tensor_tensor(out=bi_[:], in0=t3[:], in1=t4[:], op=ALU.subtract), reads=[b3, b4], writes=[bbi_])
                    ini, bini = inits[i][k % 2]
                    nin, bnin = inits[i][(k + 1) % 2]
                    mbc = prm_t[:, i, 3:4].to_broadcast([128, LC])
                    xr_, bxr_ = xpr.next(); xi_, bxi_ = xpi.next()
                    P.op("dve", lambda e, xr_=xr_, br_=br_, ini=ini, mbc=mbc: e.tensor_tensor_scan(out=xr_[:], data0=mbc, data1=br_[:], initial=ini[:, 0:1],
                                                                                          op0=ALU.mult, op1=ALU.add), reads=[bbr_, bini, bprm], writes=[bxr_])
                    P.op("dve", lambda e, xi_=xi_, bi_=bi_, ini=ini, mbc=mbc: e.tensor_tensor_scan(out=xi_[:], data0=mbc, data1=bi_[:], initial=ini[:, 1:2],
                                                                                          op0=ALU.mult, op1=ALU.add), reads=[bbi_, bini, bprm], writes=[bxi_])
                    cL, sL = COST[:, i, LC:LC + 1], SINT[:, i, LC:LC + 1]
                    lr_, li_ = xr_[:, LC - 1:LC], xi_[:, LC - 1:LC]
                    P.op("dve", lambda e, nin=nin, li_=li_, sL=sL: e.tensor_scalar(out=nin[:, 2:3], in0=li_, scalar1=sL, scalar2=None, op0=ALU.mult),
                         reads=[bxi_, btab], writes=[bnin])
                    P.op("dve", lambda e, nin=nin, lr_=lr_, cL=cL: e.scalar_tensor_tensor(out=nin[:, 0:1], in0=lr_, scalar=cL, in1=nin[:, 2:3], op0=ALU.mult, op1=ALU.subtract),
                         reads=[bxr_, btab, bnin], writes=[bnin])
                    P.op("dve", lambda e, nin=nin, li_=li_, cL=cL: e.tensor_scalar(out=nin[:, 3:4], in0=li_, scalar1=cL, scalar2=None, op0=ALU.mult),
                         reads=[bxi_, btab, bnin], writes=[bnin])
                    P.op("dve", lambda e, nin=nin, lr_=lr_, sL=sL: e.scalar_tensor_tensor(out=nin[:, 1:2], in0=lr_, scalar=sL, in1=nin[:, 3:4], op0=ALU.mult, op1=ALU.add),
                         reads=[bxr_, btab, bnin], writes=[bnin])
                    u1, c1 = u1r.next(); u2, c2 = u2r.next(); u3, c3 = u3r.next(); u4, c4 = u4r.next()
                    P.op("dve", lambda e, u1=u1, xr_=xr_, cosT=cosT: e.tensor_tensor(out=u1[:], in0=xr_[:], in1=cosT, op=ALU.mult), reads=[bxr_, btab], writes=[c1])
                    P.op("pool", lambda e, u2=u2, xi_=xi_, sinT=sinT: e.tensor_tensor(out=u2[:], in0=xi_[:], in1=sinT, op=ALU.mult), reads=[bxi_, btab], writes=[c2])
                    P.op("pool", lambda e, u3=u3, xr_=xr_, sinT=sinT: e.tensor_tensor(out=u3[:], in0=xr_[:], in1=sinT, op=ALU.mult), reads=[bxr_, btab], writes=[c3])
                    P.op("pool", lambda e, u4=u4, xi_=xi_, cosT=cosT: e.tensor_tensor(out=u4[:], in0=xi_[:], in1=cosT, op=ALU.mult), reads=[bxi_, btab], writes=[c4])
                    xr, bxr = xrr.next(); xi, bxi = xir.next()
                    P.op("dve", lambda e, xr=xr, u1=u1, u2=u2: e.tensor_tensor(out=xr[:], in0=u1[:], in1=u2[:], op=ALU.subtract), reads=[c1, c2], writes=[bxr])
                    P.op("pool", lambda e, xi=xi, u3=u3, u4=u4: e.tensor_tensor(out=xi[:], in0=u3[:], in1=u4[:], op=ALU.add), reads=[c3, c4], writes=[bxi])
                    xs.append((i, xr, bxr, xi, bxi))
                yp, byp = py.next()
                for n_, (i, xr, bxr, xi, bxi) in enumerate(xs):
                    P.op("pe", lambda e, yp=yp, i=i, xr=xr, n_=n_: e.matmul(out=yp[:, :LC], lhsT=CTr[:, i, :], rhs=xr[:], start=(n_ == 0), stop=False),
                         reads=[bCT, bxr], writes=[byp])
                    P.op("pe", lambda e, yp=yp, i=i, xi=xi, n_=n_: e.matmul(out=yp[:, :LC], lhsT=CTi[:, i, :], rhs=xi[:], start=False, stop=(n_ == 3)),
                         reads=[bCT, bxi], writes=[byp])
                y, by = yr_.next()
                P.op("dve", lambda e, y=y, yp=yp: e.scalar_tensor_tensor(out=y[:], in0=usb[:, q, tsl], scalar=dcol[:, q:q + 1], in1=yp[:, :LC],
                                                                       op0=ALU.mult, op1=ALU.add), reads=[bu_, bsm, byp], writes=[by])
                s2, bs2 = sqr_.next(); p1, bp1 = p1r.next(); p2, bp2 = p2r.next(); sg, bsg = sgr.next()
                P.op("act", lambda e, s2=s2, y=y: e.activation(out=s2[:], in_=y[:], func=AF.Square), reads=[by], writes=[bs2])
                P.op("dve", lambda e, p1=p1, s2=s2: e.tensor_scalar(out=p1[:], in0=s2[:], scalar1=0.044715, scalar2=1.0, op0=ALU.mult, op1=ALU.add),
                     reads=[bs2], writes=[bp1])
                P.op("pool", lambda e, p2=p2, p1=p1, y=y: e.tensor_tensor(out=p2[:], in0=p1[:], in1=y[:], op=ALU.mult), reads=[bp1, by], writes=[bp2])
                P.op("act", lambda e, sg=sg, p2=p2: e.activation(out=sg[:], in_=p2[:], func=AF.Sigmoid, scale=1.5957691216057308), reads=[bp2], writes=[bsg])
                P.op("dve", lambda e, sg=sg, y=y: e.tensor_tensor(out=ygsb[:, q, tsl], in0=y[:], in1=sg[:], op=ALU.mult), reads=[by, bsg], writes=[byg[k * 4 + q]])
            for o in range(4):
                zp, bzp = py.next()
                for qq in range(4):
                    P.op("pe", lambda e, zp=zp, qq=qq, o=o: e.matmul(out=zp[:, :LC], lhsT=Wgsb[:, qq, o * 128:(o + 1) * 128], rhs=ygsb[:, qq, tsl],
                                                                  start=(qq == 0), stop=(qq == 3)), reads=[bWg, byg[k * 4 + qq]], writes=[bzp])
                zs, bzs = zsg.next()
                P.op("act", lambda e, zs=zs, zp=zp, o=o: e.activation(out=zs[:], in_=zp[:, :LC], func=AF.Sigmoid, bias=bgcol[:, o:o + 1], scale=1.0),
                     reads=[bzp, bsm], writes=[bzs])
                go, bgo = gor.next()
                P.op("dve", lambda e, go=go, zs=zs, o=o: e.tensor_tensor(out=go[:], in0=ygsb[:, o, tsl], in1=zs[:], op=ALU.mult),
                     reads=[byg[k * 4 + o], bzs], writes=[bgo])
                P.dma("sp", CAT[512 + o * 128:512 + (o + 1) * 128, tsl], go[:], reads=[bgo])


def phase_dsa(P, FM, Vd, WI, CAT):
    NQ = T // 512
    with P.phase():
        ident, bident = make_identity(P)
        identb = P.sb([128, 128], BF16, "identb")
        bidb = Buf()
        P.op("dve", lambda e: e.tensor_copy(out=identb[:], in_=ident[:]), reads=[bident], writes=[bidb])
        ones32 = P.sb([128, 128], F32, "ones32")
        sel = P.sb([128, 64], F32, "sel")
        bcst = Buf()
        P.op("dve", lambda e: e.memset(ones32[:], 1.0), writes=[bcst])
        P.op("dve", lambda e: e.memset(sel[:], 0.0), writes=[bcst])
        P.op("dve", lambda e: e.memset(sel[64:65, :], 1.0), reads=[bcst], writes=[bcst])
        D0 = P.sb([128, 512], F32, "D0")
        P.op("pool", lambda e: e.iota(out=D0[:], pattern=[[1, 512]], base=0, channel_multiplier=-1,
                                      allow_small_or_imprecise_dtypes=True), writes=[bcst])
        iotaS = P.sb([128, T], F32, "iotaS")
        P.op("pool", lambda e: e.iota(out=iotaS[:], pattern=[[1, T]], base=1, channel_multiplier=0,
                                      allow_small_or_imprecise_dtypes=True), writes=[bcst])
        tcol = P.sb([128, NT], F32, "tcol")
        P.op("pool", lambda e: e.iota(out=tcol[:], pattern=[[128, NT]], base=1, channel_multiplier=1,
                                      allow_small_or_imprecise_dtypes=True), writes=[bcst])

        kisb = P.sb([128, T], BF16, "kisb")
        ksb = P.sb([128, 2, T], BF16, "ksb")
        wisb = P.sb([128, NT, 8], F32, "wisb")
        vsb = P.sb([128, NT, 4, 65], BF16, "vsb")
        bki, bk, bwi, bv = Buf(), Buf(), Buf(), Buf()
        P.dma("sp", kisb[:], FM[1920:2048, :], writes=[bki])
        for o in range(2):
            P.dma("act", ksb[:, o, :], FM[1024 + 128 * o:1024 + 128 * (o + 1), :], writes=[bk])
        P.dma("sp", wisb[:], WI.rearrange("(i p) h -> p i h", p=128), writes=[bwi])
        P.op("pool", lambda e: e.memset(vsb[:], 1.0), writes=[bv])
        Vv = Vd.rearrange("(j p) (g d) -> p j g d", p=128, g=4)
        for j0 in range(0, NT, 8):
            for g in range(4):
                P.dma("sp", vsb[:, j0:j0 + 8, g, 0:64], Vv[:, j0:j0 + 8, g, :], writes=[bv])

        acc = P.sb([128, T], F32, "acc")
        bacc = Buf()
        maskbf = P.sb([128, T], BF16, "maskbf")
        bmask = Buf()
        maskT = P.sb([128, NT, 512], BF16, "maskT")
        bmT = [Buf() for _ in range(4)]
        Dm = P.sb([128, 512], F32, "Dm")
        bDm = Buf()
        sm = P.sbring(2, [128, 16], F32, "sm")
        qir = P.sbring(2, [128, 3, 128], BF16, "qi")
        qcr = P.sbring(2, [128, 8, 512], BF16, "qc")
        rr = P.sbring(3, [128, 512], F32, "relu")
        tmr = P.sbring(3, [128, 512], F32, "tmp")
        pr = P.sbring(5, [128, 512], BF16, "p")
        dcr = P.sbring(2, [128, 512], F32, "dcj")
        dgr = P.sbring(2, [128, 128], F32, "dg")
        osr = P.sbring(2, [128, 512], F32, "osb")
        recr = P.sbring(2, [64, 512], F32, "rec")
        outr = P.sbring(2, [64, 512], BF16, "outb")
        pzi = P.psring(1, [128, 512], F32, "pzi")
        ptm = P.psring(1, [128, 512], BF16, "ptm")
        pz = P.psring(2, [128, 512], F32, "pz")
        po = P.psring(4, [128, 512], F32, "po")
        FMq = FM[0:1024, :].rearrange("(o p) t -> p o t", p=128)
        FMqi = FM[1536:1920, :].rearrange("(o p) t -> p o t", p=128)

        for c in range(NQ):
            for a in range(4):
                i = 4 * c + a
                S = 128 * (i + 1)
                qi, bqi = qir.next()
                P.dma("sp", qi[:], FMqi[:, :, i * 128:(i + 1) * 128], writes=[bqi])
                for h in range(8):
                    pb, ot = 32 * (h % 3), h // 3
                    for s0 in range(0, S, 512):
                        sn = min(512, S - s0)
                        z, bz = pzi.next()
                        P.op("pe", lambda e, z=z, qi=qi, pb=pb, ot=ot, s0=s0, sn=sn: e.matmul(
                            out=z[:, :sn], lhsT=qi[pb:pb + 32, ot, :], rhs=kisb[pb:pb + 32, s0:s0 + sn],
                            start=True, stop=True), reads=[bqi, bki], writes=[bz])
                        r, br = rr.next()
                        P.op("act", lambda e, r=r, z=z, sn=sn: e.activation(out=r[:, :sn], in_=z[:, :sn], func=AF.Relu),
                             reads=[bz], writes=[br])
                        if h == 0:
                            P.op("dve", lambda e, r=r, s0=s0, sn=sn, i=i, h=h: e.tensor_scalar(
                                out=acc[:, s0:s0 + sn], in0=r[:, :sn], scalar1=wisb[:, i, h:h + 1], scalar2=None,
                                op0=ALU.mult), reads=[br, bwi], writes=[bacc])
                        else:
                            P.op("dve", lambda e, r=r, s0=s0, sn=sn, i=i, h=h: e.scalar_tensor_tensor(
                                out=acc[:, s0:s0 + sn], in0=r[:, :sn], scalar=wisb[:, i, h:h + 1], in1=acc[:, s0:s0 + sn],
                                op0=ALU.mult, op1=ALU.add), reads=[br, bwi, bacc], writes=[bacc])
                P.op("pool", lambda e, i=i: e.affine_select(out=acc[:, i * 128:(i + 1) * 128], in_=acc[:, i * 128:(i + 1) * 128],
                                                           pattern=[[-1, 128]], compare_op=ALU.is_ge, fill=P.freg(-BIG), base=0,
                                                           channel_multiplier=1), reads=[bacc], writes=[bacc])
                s_, bs_ = sm.next()
                if i >= 2:
                    P.op("dve", lambda e, s_=s_, S=S: e.tensor_reduce(out=s_[:, 1:2], in_=acc[:, 0:S], axis=AX.X, op=ALU.max),
                         reads=[bacc], writes=[bs_])
                    P.op("dve", lambda e, s_=s_, i=i: e.tensor_reduce(out=s_[:, 0:1], in_=acc[:, 0:128 * i], axis=AX.X, op=ALU.min),
                         reads=[bacc, bs_], writes=[bs_])
                    P.op("dve", lambda e, s_=s_: e.tensor_tensor(out=s_[:, 5:6], in0=s_[:, 1:2], in1=s_[:, 0:1], op=ALU.subtract),
                         reads=[bs_], writes=[bs_])
                    for it in range(NBISECT):
                        hw = 2.0 ** (-(it + 1))
                        P.op("dve", lambda e, s_=s_, hw=hw: e.scalar_tensor_tensor(out=s_[:, 2:3], in0=s_[:, 5:6], scalar=hw, in1=s_[:, 0:1],
                                                                                 op0=ALU.mult, op1=ALU.add), reads=[bs_], writes=[bs_])
                        P.op("dve", lambda e, s_=s_, S=S: e.tensor_scalar(out=maskbf[:, 0:S], in0=acc[:, 0:S], scalar1=s_[:, 2:3], scalar2=0.0,
                                                                        op0=ALU.is_ge, op1=ALU.add, accum_out=s_[:, 3:4]),
                             reads=[bacc, bs_, bmask], writes=[bmask, bs_])
                        P.op("dve", lambda e, s_=s_: e.scalar_tensor_tensor(out=s_[:, 4:5], in0=s_[:, 3:4], scalar=float(TOPK), in1=s_[:, 5:6],
                                                                            op0=ALU.is_ge, op1=ALU.mult), reads=[bs_], writes=[bs_])
                        P.op("dve", lambda e, s_=s_, hw=hw: e.scalar_tensor_tensor(out=s_[:, 0:1], in0=s_[:, 4:5], scalar=hw, in1=s_[:, 0:1],
                                                                                 op0=ALU.mult, op1=ALU.add), reads=[bs_], writes=[bs_])
                    P.op("dve", lambda e, s_=s_, S=S: e.tensor_scalar(out=maskbf[:, 0:S], in0=acc[:, 0:S], scalar1=s_[:, 0:1], scalar2=None,
                                                                    op0=ALU.is_lt), reads=[bacc, bs_, bmask], writes=[bmask])
                else:
                    P.op("dve", lambda e, S=S: e.tensor_scalar(out=maskbf[:, 0:S], in0=acc[:, 0:S], scalar1=-0.5 * BIG, scalar2=None,
                                                             op0=ALU.is_lt), reads=[bacc, bmask], writes=[bmask])
                P.op("dve", lambda e, S=S: e.scalar_tensor_tensor(out=acc[:, 0:S], in0=maskbf[:, 0:S], scalar=-float(T + 1), in1=iotaS[:, 0:S],
                                                                 op0=ALU.mult, op1=ALU.add), reads=[bmask, bcst, bacc], writes=[bacc])
                P.op("dve", lambda e, s_=s_, S=S: e.tensor_reduce(out=s_[:, 7:8], in_=acc[:, 0:S], axis=AX.X, op=ALU.max),
                     reads=[bacc, bs_], writes=[bs_])
                P.op("dve", lambda e, s_=s_, i=i: e.tensor_tensor(out=s_[:, 8:9], in0=tcol[:, i:i + 1], in1=s_[:, 7:8], op=ALU.subtract),
                     reads=[bs_, bcst], writes=[bs_])
                dg, bdg = dgr.next()
                P.op("dve", lambda e, dg=dg, s_=s_: e.tensor_scalar(out=dg[:], in0=ident[:], scalar1=s_[:, 8:9], scalar2=None, op0=ALU.mult),
                     reads=[bident, bs_], writes=[bdg])
                zb, bzb = pzi.next()
                P.op("pe", lambda e, zb=zb, dg=dg: e.matmul(out=zb[:, 0:128], lhsT=ones32[:], rhs=dg[:], start=True, stop=True),
                     reads=[bcst, bdg], writes=[bzb])
                P.op("dve", lambda e, zb=zb, a=a: e.tensor_tensor(out=Dm[:, a * 128:(a + 1) * 128], in0=D0[:, a * 128:(a + 1) * 128],
                                                                 in1=zb[:, 0:128], op=ALU.subtract), reads=[bzb, bcst, bDm], writes=[bDm])
                for j0 in range(0, i + 1, 4):
                    jn = min(4, i + 1 - j0)
                    pt, bpt = ptm.next()
                    for jj in range(jn):
                        j = j0 + jj
                        P.op("pe", lambda e, pt=pt, jj=jj, j=j: e.transpose(out=pt[:, jj * 128:(jj + 1) * 128],
                                                                           in_=maskbf[:, j * 128:(j + 1) * 128], identity=identb[:]),
                             reads=[bmask, bidb], writes=[bpt])
                    o_ = maskT[:, j0:j0 + jn, a * 128:(a + 1) * 128]
                    i_ = pt[:, 0:jn * 128].rearrange("p (k t) -> p k t", k=jn)
                    P.op("act", lambda e, o_=o_, i_=i_: e.copy(out=o_, in_=i_), reads=[bpt], writes=[bmT[a]])
                if i + 1 <= 4 * c + 3:
                    P.op("pool", lambda e, i=i, a=a, c=c: e.memset(maskT[:, i + 1:4 * c + 4, a * 128:(a + 1) * 128], 1.0), writes=[bmT[a]])

            qc, bqc = qcr.next()
            P.dma("sp", qc[:], FMq[:, :, c * 512:(c + 1) * 512], writes=[bqc])
            jn_all = 4 * c + 4
            units = []
            for g in range(4):
                Obs = [po.next() for _ in range(4)]
                for j in range(jn_all):
                    for r in range(4):
                        units.append(dict(g=g, r=r, j=j, O=Obs[r]))
            dcj_cache = {}

            def get_dcj(j):
                if j in dcj_cache:
                    return dcj_cache[j]
                d, bd = dcr.next()
                off = float(512 * c - 128 * j)
                P.op("dve", lambda e, d=d, off=off: e.tensor_scalar(out=d[:], in0=Dm[:], scalar1=off, scalar2=0.0, op0=ALU.add, op1=ALU.max),
                     reads=[bDm], writes=[bd])
                P.op("dve", lambda e, d=d, j=j: e.scalar_tensor_tensor(out=d[:], in0=maskT[:, j, :], scalar=MASKBIG, in1=d[:],
                                                                      op0=ALU.mult, op1=ALU.add), reads=[bd] + bmT, writes=[bd])
                dcj_cache.clear()
                dcj_cache[j] = (d, bd)
                return d, bd

            def B0(u):
                g, r, j = u["g"], u["r"], u["j"]
                pb, kt, qt = 64 * (g % 2), g // 2, 4 * (g // 2) + r
                z, bz = pz.next()
                P.op("pe", lambda e: e.matmul(out=z[:], lhsT=ksb[pb:pb + 64, kt, j * 128:(j + 1) * 128], rhs=qc[pb:pb + 64, qt, :],
                                              start=True, stop=True), reads=[bk, bqc], writes=[bz])
                u.update(z=z, bz=bz)

            def B1(u):
                g, r, j = u["g"], u["r"], u["j"]
                h = 4 * g + r
                slope = 2.0 ** (-8.0 * (h + 1) / 16.0)
                z, bz = u["z"], u["bz"]
                d, bd = get_dcj(j)
                tm, btm = tmr.next()
                P.op("dve", lambda e: e.scalar_tensor_tensor(out=tm[:], in0=d[:], scalar=-slope, in1=z[:], op0=ALU.mult, op1=ALU.add),
                     reads=[bd, bz], writes=[btm])
                u.update(tm=tm, btm=btm)

            def B2(u):
                tm, btm = u["tm"], u["btm"]
                p, bp = pr.next()
                P.op("act", lambda e: e.activation(out=p[:], in_=tm[:], func=AF.Exp), reads=[btm], writes=[bp])
                u.update(pm=p, bpm=bp)

            def B3(u):
                g, r, j = u["g"], u["r"], u["j"]
                h = 4 * g + r
                O, bO = u["O"]
                pm, bpm = u["pm"], u["bpm"]
                P.op("pe", lambda e: e.matmul(out=O[0:65, :], lhsT=vsb[:, j, g, :], rhs=pm[:], start=(j == 0), stop=(j == jn_all - 1)),
                     reads=[bpm, bv], writes=[bO])
                if j == jn_all - 1:
                    osb, bos = osr.next()
                    P.op("act", lambda e: e.copy(out=osb[0:65, :], in_=O[0:65, :]), reads=[bO], writes=[bos])
                    dn, bdn = pzi.next()
                    P.op("pe", lambda e: e.matmul(out=dn[0:64, :], lhsT=sel[0:65, :], rhs=osb[0:65, :], start=True, stop=True),
                         reads=[bos, bcst], writes=[bdn])
                    rec, brec = recr.next()
                    P.op("dve", lambda e: e.reciprocal(out=rec[:], in_=dn[0:64, :]), reads=[bdn], writes=[brec])
                    ob, bob = outr.next()
                    P.op("pool", lambda e: e.tensor_tensor(out=ob[:], in0=osb[0:64, :], in1=rec[:], op=ALU.mult), reads=[bos, brec], writes=[bob])
                    P.dma("sp", CAT[h * 64:(h + 1) * 64, c * 512:(c + 1) * 512], ob[:], reads=[bob])

            stages = [B0, B1, B2, B3]
            n = len(units)
            for idx in range(n + len(stages) - 1):
                for k in range(len(stages) - 1, -1, -1):
                    if 0 <= idx - k < n:
                        stages[k](units[idx - k])


IN_SHAPES = {
    "x": [T, D], "sb_ssm_w_in": [2, D, 2048], "ssm_log_dt": [2, 32], "ssm_lam_re": [2, 32, 64], "ssm_lam_im": [2, 32, 64],
    "ssm_b_re": [2, 32, 64, 16], "ssm_b_im": [2, 32, 64, 16], "ssm_c_re": [2, 32, 16, 64], "ssm_c_im": [2, 32, 16, 64],
    "ssm_d": [2, 32, 16], "ssm_w_glu": [2, 512, 512], "ssm_b_glu": [2, 512], "sb_ssm_w_out": [2, D, D],
    "dsa_w_in": [2, D, 1832], "dsa_w_out": [2, D, D], "ln_mix_g": [4, D], "ln_mix_b": [4, D], "ln_ffn_g": [4, D],
    "ln_ffn_b": [4, D], "mlp_w1": [4, D, DFF], "mlp_w2": [4, DFF, D],
}
DSA_NCOLS = 2312


def dsa_colmap(dst):
    cm = []
    for gam in range(2):
        for r in range(4):
            tau = 4 * gam + r
            cm.append((64 * (8 * gam + r), 64, dst, 128 * tau))
            cm.append((64 * (8 * gam + 4 + r), 64, dst, 128 * tau + 64))
    cm.append((1024, 256, dst, 1024))
    for h in range(8):
        cm.append((1536 + 32 * h, 32, dst, 1536 + 128 * (h // 3) + 32 * (h % 3)))
    for rep in range(3):
        cm.append((1792, 32, dst, 1920 + 32 * rep))
    cm.append((1280, 256, dst, 2048))
    cm.append((1824, 8, dst, 2304))
    return cm


def build_program(nlayers=DEPTH):
    nc = bass.Bass("TRN2", target_bir_lowering=False)
    I = {k: nc.dram_tensor(k, v, F32, kind="ExternalInput").ap() for k, v in IN_SHAPES.items()}
    y = nc.dram_tensor("y", [T, D], F32, kind="ExternalOutput").ap()

    def scr(name, shape, dt):
        return nc.dram_tensor(name, shape, dt, kind="Internal").ap()
    XT = scr("XT", [D, T], BF16)
    X1 = scr("X1", [T, D], F32)
    X1T = scr("X1T", [D, T], BF16)
    XB = [scr("XB0", [T, D], F32), scr("XB1", [T, D], F32)]
    FM = scr("FM", [2048, T], BF16)
    TMV = scr("TMV", [T, 512], BF16)
    WIs = scr("WIs", [T, 8], F32)
    CAT = scr("CAT", [D, T], BF16)
    WIN = [scr("WIN%d" % l, [D, DSA_NCOLS], BF16) for l in range(nlayers)]
    WOUT = [scr("WOUT%d" % l, [D, D], BF16) for l in range(nlayers)]
    W1 = [scr("W1_%d" % l, [D, DFF], BF16) for l in range(nlayers)]
    W2 = [scr("W2_%d" % l, [DFF, D], BF16) for l in range(nlayers)]
    WG = [scr("WG%d" % j, [512, 512], BF16) for j in range(2)]

    with ExitStack() as es:
        P = Prog(nc, es)
        jobs = []
        for l in range(nlayers):
            j = l // 2
            if l % 2 == 0:
                jobs.append((I["sb_ssm_w_in"][j], [(0, 2048, WIN[l], 0)]))
                jobs.append((I["sb_ssm_w_out"][j], [(0, D, WOUT[l], 0)]))
                jobs.append((I["ssm_w_glu"][j], [(0, 512, WG[j], 0)]))
            else:
                jobs.append((I["dsa_w_in"][j], dsa_colmap(WIN[l])))
                jobs.append((I["dsa_w_out"][j], [(0, D, WOUT[l], 0)]))
            jobs.append((I["mlp_w1"][l], [(0, DFF, W1[l], 0)]))
            jobs.append((I["mlp_w2"][l], [(0, D, W2[l], 0)]))
        phase_cast(P, jobs)
        phase_transpose(P, I["x"], XT)
        xin = I["x"]
        for l in range(nlayers):
            j = l // 2
            last = (l == nlayers - 1)
            if l % 2 == 0:
                fm = [(128 * o, FM, 128 * o) for o in range(8)] + [(128 * o, FM, 1024 + 128 * (o - 12)) for o in range(12, 16)]
                phase_inproj(P, XT, WIN[l], 2048, fm, [(1024, 512, TMV, BF16)])
                phase_sb_attn(P, FM[0:1024, :], TMV, CAT)
                prm = (I["ssm_log_dt"][j], I["ssm_lam_re"][j], I["ssm_lam_im"][j], I["ssm_b_re"][j], I["ssm_b_im"][j],
                       I["ssm_c_re"][j], I["ssm_c_im"][j], I["ssm_d"][j], I["ssm_b_glu"][j])
                phase_ssm(P, FM[1024:1536, :], prm, WG[j], CAT)
            else:
                fm = [(128 * o, FM, 128 * o, 0.125) for o in range(8)] + [(128 * o, FM, 128 * o) for o in (8, 9, 12, 13, 14, 15)]
                phase_inproj(P, XT, WIN[l], DSA_NCOLS, fm, [(2048, 256, TMV[:, 0:256], BF16), (2304, 8, WIs, F32)])
                phase_dsa(P, FM, TMV[:, 0:256], WIs, CAT)
            phase_outproj_ln(P, CAT, WOUT[l], xin, I["ln_mix_g"][l:l + 1, :], I["ln_mix_b"][l:l + 1, :], X1, X1T)
            xout = y if last else XB[l % 2]
            phase_mlp(P, X1T, X1, W1[l], W2[l], I["ln_ffn_g"][l:l + 1, :], I["ln_ffn_b"][l:l + 1, :], xout,
                      None if last else XT)
            xin = xout
        P.barrier()
    return nc


_PROG = {}


def kernel(**inputs):
    if "nc" not in _PROG:
        _PROG["nc"] = build_program()
    nc = _PROG["nc"]
    x = np.ascontiguousarray(np.asarray(inputs["x"], dtype=np.float32))
    B = x.shape[0]
    shared = {k: np.ascontiguousarray(np.asarray(inputs[k], dtype=np.float32)) for k in IN_SHAPES if k != "x"}
    in_maps = []
    for b in range(B):
        m = dict(shared)
        m["x"] = x[b]
        in_maps.append(m)
    res = run_bass_kernel_spmd(nc, in_maps, core_ids=[4 + b for b in range(B)])
    return np.stack([np.asarray(r["y"], dtype=np.float32) for r in res.results], axis=0)
```

```python
import math
from contextlib import ExitStack, contextmanager

import numpy as np
import concourse.bass as bass
import concourse.mybir as mybir
from concourse.bass_utils import run_bass_kernel_spmd

F32 = mybir.dt.float32
BF16 = mybir.dt.bfloat16
I32 = mybir.dt.int32
AF = mybir.ActivationFunctionType
ALU = mybir.AluOpType
AX = mybir.AxisListType

D = 1024
T = 4096
NT = T // 128
DEPTH = 4
DFF = 4096
ALPHA = (2 * DEPTH) ** 0.25
LN_EPS = 1e-5
NCORES = 4
TOPK = 256
BIG = 1.0e30
NBISECT = 22
MASKBIG = 131072.0


class Buf:
    __slots__ = ("name", "w", "r")

    def __init__(self, name="b"):
        self.name = name
        self.w = None
        self.r = {}


class Ring:
    def __init__(self, items):
        self.items = items
        self.i = 0

    def next(self):
        it = self.items[self.i % len(self.items)]
        self.i += 1
        return it


class Prog:
    ENG = ("pe", "act", "dve", "pool", "sp")
    NDMA = 40

    def __init__(self, nc, es):
        self.nc = nc
        self.es = es
        self.eng = {"pe": nc.tensor, "act": nc.scalar, "dve": nc.vector,
                    "pool": nc.gpsimd, "sp": nc.sync}
        self.es_root = es
        self.epoch = 0
        self.key = {e: e + "#0" for e in self.ENG}
        self.sem = {e: es.enter_context(nc.semaphore("s_" + e)) for e in self.ENG}
        self.cnt = {e: 0 for e in self.ENG}
        self.dsem = [es.enter_context(nc.semaphore("d%d" % i)) for i in range(self.NDMA)]
        self.dcnt = [0] * self.NDMA
        self.dnext = 0
        self.seen = {e: {} for e in self.ENG}
        self.nins = 0
        self.uid = 0
        self._fregs = {}

    def freg(self, v):
        v = float(v)
        if v not in self._fregs:
            self._fregs[v] = self.nc.gpsimd.to_reg(v)
        return self._fregs[v]

    def sb(self, shape, dt, name=None):
        self.uid += 1
        return self.es.enter_context(self.nc.sbuf_tensor("%s_%d" % (name or "t", self.uid), list(shape), dt))

    def ps(self, shape, dt=F32, name=None):
        self.uid += 1
        return self.es.enter_context(self.nc.psum_tensor("%s_%d" % (name or "p", self.uid), list(shape), dt))

    def sbring(self, n, shape, dt, name=None):
        return Ring([(self.sb(shape, dt, name), Buf()) for _ in range(n)])

    def psring(self, n, shape, dt=F32, name=None):
        return Ring([(self.ps(shape, dt, name), Buf()) for _ in range(n)])

    @contextmanager
    def phase(self):
        old = self.es
        with ExitStack() as st:
            self.es = st
            yield
            self.barrier()
            if max(self.cnt.values()) > 9000:
                self.new_epoch()
        self.es = old

    def new_epoch(self):
        self.epoch += 1
        for e in ("pe", "act", "dve", "pool"):
            if self.cnt[e] == 0:
                continue
            self.sem[e] = self.es_root.enter_context(self.nc.semaphore("s_%s_%d" % (e, self.epoch)))
            self.cnt[e] = 0
            self.key[e] = "%s#%d" % (e, self.epoch)

    def _deps(self, reads, writes):
        evs = []
        for b in reads:
            if b.w is not None:
                evs.append(b.w)
        for b in writes:
            if b.w is not None:
                evs.append(b.w)
            evs.extend(b.r.values())
        return evs

    def _wait(self, e, evs):
        best = {}
        for (k, s, v) in evs:
            if self.seen[e].get(k, 0) >= v:
                continue
            if k not in best or best[k][1] < v:
                best[k] = (s, v)
        for k, (s, v) in best.items():
            self.seen[e][k] = v
            self.eng[e].wait_ge(s, v)

    def _record(self, ev, reads, writes):
        k = ev[0]
        for b in reads:
            b.r[k] = ev
        for b in writes:
            b.w = ev
            b.r = {}

    def op(self, e, fn, reads=(), writes=()):
        evs = self._deps(reads, writes)
        if e == "pe":
            evs = [x for x in evs if not x[0].startswith("pe#")]
        self._wait(e, evs)
        ins = fn(self.eng[e])
        self.cnt[e] += 1
        ins.then_inc(self.sem[e], 1)
        ev = (self.key[e], self.sem[e], self.cnt[e])
        self._record(ev, reads, writes)
        self.nins += 1
        return ev

    def dma(self, qe, out, in_, reads=(), writes=(), **kw):
        evs = self._deps(reads, writes)
        i = self.dnext
        self.dnext = (self.dnext + 1) % self.NDMA
        key = "d%d" % i
        if self.dcnt[i] > 0:
            evs = list(evs) + [(key, self.dsem[i], self.dcnt[i])]
        self._wait(qe, evs)
        self.dcnt[i] += 16
        ins = self.eng[qe].dma_start(out=out, in_=in_, **kw)
        ins.then_inc(self.dsem[i], 16)
        ev = (key, self.dsem[i], self.dcnt[i])
        self._record(ev, reads, writes)
        self.nins += 1
        return ev

    def barrier(self):
        evs = [(self.key[e], self.sem[e], self.cnt[e]) for e in self.ENG if self.cnt[e] > 0]
        evs += [("d%d" % i, self.dsem[i], self.dcnt[i]) for i in range(self.NDMA) if self.dcnt[i] > 0]
        for e in self.ENG:
            self._wait(e, evs)


def make_identity(P, dt=F32):
    ident = P.sb([128, 128], F32, "ident")
    b = Buf()
    P.op("pool", lambda e: e.memset(ident[:], 1.0), writes=[b])
    P.op("pool", lambda e: e.affine_select(out=ident[:], in_=ident[:], pattern=[[-1, 128]],
                                           compare_op=ALU.is_equal, fill=P.freg(0.0), base=0,
                                           channel_multiplier=1), reads=[b], writes=[b])
    if dt == F32:
        return ident, b
    idb = P.sb([128, 128], dt, "identb")
    bb = Buf()
    P.op("dve", lambda e: e.tensor_copy(out=idb[:], in_=ident[:]), reads=[b], writes=[bb])
    return idb, bb


def cast_engine_op(P, k, out, in_, reads, writes):
    e = ("dve", "pool", "act")[k % 3]
    if e == "act":
        return P.op("act", lambda g: g.copy(out=out, in_=in_), reads=reads, writes=writes)
    return P.op(e, lambda g: g.tensor_copy(out=out, in_=in_), reads=reads, writes=writes)


def phase_cast(P, jobs):
    CH = 2048
    with P.phase():
        st32 = P.sbring(3, [128, CH], F32, "st32")
        st16 = P.sbring(3, [128, CH], BF16, "st16")
        k = 0
        for (src, cmap) in jobs:
            R, C = src.shape
            for r0 in range(0, R, 128):
                for c0 in range(0, C, CH):
                    cn = min(CH, C - c0)
                    a, ba = st32.next()
                    b, bb = st16.next()
                    P.dma("sp", a[:, :cn], src[r0:r0 + 128, c0:c0 + cn], writes=[ba])
                    cast_engine_op(P, k, b[:, :cn], a[:, :cn], [ba], [bb])
                    k += 1
                    for (s0, n, dst, d0) in cmap:
                        lo = max(s0, c0)
                        hi = min(s0 + n, c0 + cn)
                        if lo >= hi:
                            continue
                        P.dma("act" if (k % 2) else "sp", dst[r0:r0 + 128, d0 + lo - s0:d0 + hi - s0],
                              b[:, lo - c0:hi - c0], reads=[bb])


def transpose_store(P, ident, bident, src, bsrc, a, stage, bstage, ptr):
    for half in range(2):
        pt, bpt = ptr.next()
        for kk in range(4):
            k = half * 4 + kk
            P.op("pe", lambda e, k=k, kk=kk, pt=pt: e.transpose(out=pt[:, kk * 128:(kk + 1) * 128],
                                                            in_=src[:, k * 128:(k + 1) * 128],
                                                            identity=ident[:]),
                 reads=[bsrc, bident], writes=[bpt])
        o = stage[:, half * 4:(half + 1) * 4, a * 128:(a + 1) * 128]
        i = pt[:].rearrange("p (k t) -> p k t", k=4)
        if half == 0:
            P.op("act", lambda e, o=o, i=i: e.copy(out=o, in_=i), reads=[bpt], writes=[bstage])
        else:
            P.op("dve", lambda e, o=o, i=i: e.tensor_copy(out=o, in_=i), reads=[bpt], writes=[bstage])


def phase_transpose(P, x_tm, xT):
    with P.phase():
        ident, bident = make_identity(P)
        xin = P.sbring(3, [128, D], F32, "xin")
        stg = P.sbring(2, [128, 8, 512], BF16, "stg")
        ptr = P.psring(4, [128, 512], F32, "ptr")
        for c in range(T // 512):
            stage, bstage = stg.next()
            for a in range(4):
                xt, bx = xin.next()
                t0 = c * 512 + a * 128
                P.dma("sp", xt[:], x_tm[t0:t0 + 128, :], writes=[bx])
                transpose_store(P, ident, bident, xt, bx, a, stage, bstage, ptr)
            P.dma("sp", xT.rearrange("(k p) t -> p k t", p=128)[:, :, c * 512:(c + 1) * 512],
                  stage[:], reads=[bstage])


def phase_inproj(P, xT, W, ncols, fm_tiles, tm_specs):
    with P.phase():
        Wsb = P.sb([128, 8, ncols], BF16, "Wsb")
        bW = Buf()
        for k in range(8):
            P.dma("sp" if k % 2 else "act", Wsb[:, k, :], W[k * 128:(k + 1) * 128, 0:ncols], writes=[bW])
        xr = P.sbring(2, [128, 8, 512], BF16, "xTc")
        pr = P.psring(4, [128, 512], F32, "pp")
        orr = P.sbring(4, [128, 512], BF16, "ofm")
        otm = {}
        for (c0, n, dst, dt) in tm_specs:
            otm[c0] = P.sbring(3, [128, n], dt, "otm")
        xTv = xT.rearrange("(k p) t -> p k t", p=128)
        cnt = 0
        for c in range(T // 512):
            xc, bxc = xr.next()
            P.dma("sp", xc[:], xTv[:, :, c * 512:(c + 1) * 512], writes=[bxc])
            for ft in fm_tiles:
                c0, dst, r0 = ft[0], ft[1], ft[2]
                scl = ft[3] if len(ft) > 3 else 1.0
                pt, bpt = pr.next()
                for k in range(8):
                    P.op("pe", lambda e, k=k, pt=pt, c0=c0, xc=xc: e.matmul(
                        out=pt[:], lhsT=Wsb[:, k, c0:c0 + 128], rhs=xc[:, k, :],
                        start=(k == 0), stop=(k == 7)), reads=[bW, bxc], writes=[bpt])
                ot, bot = orr.next()
                if cnt % 2 == 0:
                    P.op("act", lambda e, ot=ot, pt=pt, scl=scl: e.mul(out=ot[:], in_=pt[:], mul=scl), reads=[bpt], writes=[bot])
                else:
                    P.op("dve", lambda e, ot=ot, pt=pt, scl=scl: e.tensor_scalar(out=ot[:], in0=pt[:], scalar1=scl, scalar2=None,
                                                                              op0=ALU.mult), reads=[bpt], writes=[bot])
                cnt += 1
                P.dma("sp", dst[r0:r0 + 128, c * 512:(c + 1) * 512], ot[:], reads=[bot])
            for (c0, n, dst, dt) in tm_specs:
                for a in range(4):
                    pt, bpt = pr.next()
                    for k in range(8):
                        P.op("pe", lambda e, k=k, pt=pt, c0=c0, n=n, a=a, xc=xc: e.matmul(
                            out=pt[:, :n], lhsT=xc[:, k, a * 128:(a + 1) * 128], rhs=Wsb[:, k, c0:c0 + n],
                            start=(k == 0), stop=(k == 7)), reads=[bW, bxc], writes=[bpt])
                    ot, bot = otm[c0].next()
                    if cnt % 2 == 0:
                        P.op("act", lambda e, ot=ot, pt=pt, n=n: e.copy(out=ot[:], in_=pt[:, :n]), reads=[bpt], writes=[bot])
                    else:
                        P.op("dve", lambda e, ot=ot, pt=pt, n=n: e.tensor_copy(out=ot[:], in_=pt[:, :n]), reads=[bpt], writes=[bot])
                    cnt += 1
                    t0 = c * 512 + a * 128
                    P.dma("sp", dst[t0:t0 + 128, :], ot[:], reads=[bot])


def ln_tile(P, r, br, gbc, bbc, bgb, scr):
    st, bst = scr["st"].next()
    mv, bmv = scr["mv"].next()
    for h in range(2):
        P.op("dve", lambda e, h=h: e.bn_stats(out=st[:, h * 6:(h + 1) * 6], in_=r[:, h * 512:(h + 1) * 512]),
             reads=[br], writes=[bst])
    P.op("dve", lambda e: e.bn_aggr(out=mv[:, 0:2], in_=st[:, 0:12]), reads=[bst], writes=[bmv])
    P.op("dve", lambda e: e.tensor_scalar(out=mv[:, 2:3], in0=mv[:, 1:2], scalar1=LN_EPS, scalar2=None,
                                          op0=ALU.add), reads=[bmv], writes=[bmv])
    P.op("act", lambda e: e.activation(out=mv[:, 3:4], in_=mv[:, 2:3], func=AF.Sqrt), reads=[bmv], writes=[bmv])
    P.op("dve", lambda e: e.reciprocal(out=mv[:, 4:5], in_=mv[:, 3:4]), reads=[bmv], writes=[bmv])
    P.op("dve", lambda e: e.tensor_scalar(out=r[:], in0=r[:], scalar1=mv[:, 0:1], scalar2=mv[:, 4:5],
                                          op0=ALU.subtract, op1=ALU.mult), reads=[br, bmv], writes=[br])
    P.op("pool", lambda e: e.tensor_tensor(out=r[:], in0=r[:], in1=gbc[:], op=ALU.mult), reads=[br, bgb], writes=[br])
    P.op("dve", lambda e: e.tensor_tensor(out=r[:], in0=r[:], in1=bbc[:], op=ALU.add), reads=[br, bgb], writes=[br])


def load_gb(P, g_ap, b_ap):
    gbc = P.sb([128, D], F32, "gbc")
    bbc = P.sb([128, D], F32, "bbc")
    bgb = Buf()
    P.dma("sp", gbc[:], g_ap.to_broadcast([128, D]), writes=[bgb])
    P.dma("sp", bbc[:], b_ap.to_broadcast([128, D]), writes=[bgb])
    return gbc, bbc, bgb


def ln_scratch(P):
    return {"st": P.sbring(3, [128, 12], F32, "lnst"), "mv": P.sbring(3, [128, 8], F32, "lnmv")}


def phase_outproj_ln(P, catT, Wout, x_tm, g_ap, b_ap, x1_tm, x1T):
    with P.phase():
        ident, bident = make_identity(P)
        Wsb = P.sb([128, 8, D], BF16, "Wo")
        bW = Buf()
        for k in range(8):
            P.dma("sp" if k % 2 else "act", Wsb[:, k, :], Wout[k * 128:(k + 1) * 128, :], writes=[bW])
        gbc, bbc, bgb = load_gb(P, g_ap, b_ap)
        scr = ln_scratch(P)
        cr = P.sbring(2, [128, 8, 512], BF16, "catc")
        xr = P.sbring(3, [128, D], F32, "xres")
        rr = P.sbring(3, [128, D], F32, "rr")
        stg = P.sbring(2, [128, 8, 512], BF16, "stg")
        pr = P.psring(4, [128, 512], F32, "pp")
        ptr = P.psring(4, [128, 512], F32, "ptr")
        cv = catT.rearrange("(k p) t -> p k t", p=128)
        for c in range(T // 512):
            cc, bcc = cr.next()
            P.dma("sp", cc[:], cv[:, :, c * 512:(c + 1) * 512], writes=[bcc])
            stage, bstage = stg.next()
            for a in range(4):
                t0 = c * 512 + a * 128
                xt, bx = xr.next()
                P.dma("sp", xt[:], x_tm[t0:t0 + 128, :], writes=[bx])
                r, br = rr.next()
                for oc in range(2):
                    pt, bpt = pr.next()
                    for k in range(8):
                        P.op("pe", lambda e, k=k, pt=pt, oc=oc, a=a, cc=cc: e.matmul(
                            out=pt[:], lhsT=cc[:, k, a * 128:(a + 1) * 128], rhs=Wsb[:, k, oc * 512:(oc + 1) * 512],
                            start=(k == 0), stop=(k == 7)), reads=[bW, bcc], writes=[bpt])
                    P.op("dve", lambda e, pt=pt, oc=oc, r=r, xt=xt: e.scalar_tensor_tensor(
                        out=r[:, oc * 512:(oc + 1) * 512], in0=xt[:, oc * 512:(oc + 1) * 512], scalar=ALPHA,
                        in1=pt[:], op0=ALU.mult, op1=ALU.add), reads=[bpt, bx], writes=[br])
                ln_tile(P, r, br, gbc, bbc, bgb, scr)
                P.dma("sp", x1_tm[t0:t0 + 128, :], r[:], reads=[br])
                transpose_store(P, ident, bident, r, br, a, stage, bstage, ptr)
            P.dma("sp", x1T.rearrange("(k p) t -> p k t", p=128)[:, :, c * 512:(c + 1) * 512],
                  stage[:], reads=[bstage])


def phase_mlp(P, x1T, x1_tm, W1, W2, g_ap, b_ap, x2_tm, x2T):
    with P.phase():
        ident, bident = make_identity(P)
        W1sb = P.sb([128, 8, DFF], BF16, "W1")
        bW1 = Buf()
        for k in range(8):
            P.dma("sp" if k % 2 else "act", W1sb[:, k, :], W1[k * 128:(k + 1) * 128, :], writes=[bW1])
        gbc, bbc, bgb = load_gb(P, g_ap, b_ap)
        scr = ln_scratch(P)
        xr = P.sbring(2, [128, 8, 512], BF16, "x1c")
        hT = P.sb([128, 32, 512], BF16, "hT")
        bh = [Buf() for _ in range(32)]
        sq = P.sbring(3, [128, 512], F32, "sq")
        w2r = P.sbring(4, [128, 512], BF16, "w2")
        xres = P.sbring(2, [128, D], F32, "xres")
        rr = [(P.sb([128, D], F32, "rr"), Buf()) for _ in range(4)]
        stg = P.sbring(1, [128, 8, 512], BF16, "stg")
        ph = P.psring(2, [128, 512], F32, "ph")
        pacc = [(P.ps([128, 512], F32, "acc"), Buf()) for _ in range(4)]
        ptr = P.psring(2, [128, 512], F32, "ptr")
        xv = x1T.rearrange("(k p) t -> p k t", p=128)
        for c in range(T // 512):
            xc, bxc = xr.next()
            P.dma("sp", xc[:], xv[:, :, c * 512:(c + 1) * 512], writes=[bxc])
            for f in range(32):
                pt, bpt = ph.next()
                for k in range(8):
                    P.op("pe", lambda e, k=k, pt=pt, f=f, xc=xc: e.matmul(
                        out=pt[:], lhsT=W1sb[:, k, f * 128:(f + 1) * 128], rhs=xc[:, k, :],
                        start=(k == 0), stop=(k == 7)), reads=[bW1, bxc], writes=[bpt])
                s, bs = sq.next()
                P.op("act", lambda e, s=s, pt=pt: e.activation(out=s[:], in_=pt[:], func=AF.Square),
                     reads=[bpt], writes=[bs])
                P.op("dve", lambda e, s=s, pt=pt, f=f: e.scalar_tensor_tensor(
                    out=hT[:, f, :], in0=pt[:], scalar=0.0, in1=s[:], op0=ALU.is_gt, op1=ALU.mult),
                    reads=[bpt, bs], writes=[bh[f]])
            for oc in range(2):
                for f in range(32):
                    w2, bw2 = w2r.next()
                    P.dma("sp" if f % 2 else "act", w2[:], W2[f * 128:(f + 1) * 128, oc * 512:(oc + 1) * 512], writes=[bw2])
                    for a in range(4):
                        P.op("pe", lambda e, a=a, f=f, w2=w2: e.matmul(
                            out=pacc[a][0][:], lhsT=hT[:, f, a * 128:(a + 1) * 128], rhs=w2[:],
                            start=(f == 0), stop=(f == 31)), reads=[bh[f], bw2], writes=[pacc[a][1]])
                for a in range(4):
                    t0 = c * 512 + a * 128
                    xt, bx = xres.next()
                    P.dma("sp", xt[:, :512], x1_tm[t0:t0 + 128, oc * 512:(oc + 1) * 512], writes=[bx])
                    r, br = rr[a]
                    P.op("dve", lambda e, a=a, oc=oc, r=r, xt=xt: e.scalar_tensor_tensor(
                        out=r[:, oc * 512:(oc + 1) * 512], in0=xt[:, :512], scalar=ALPHA,
                        in1=pacc[a][0][:], op0=ALU.mult, op1=ALU.add), reads=[pacc[a][1], bx], writes=[br])
            stage, bstage = stg.next()
            for a in range(4):
                t0 = c * 512 + a * 128
                r, br = rr[a]
                ln_tile(P, r, br, gbc, bbc, bgb, scr)
                P.dma("sp", x2_tm[t0:t0 + 128, :], r[:], reads=[br])
                if x2T is not None:
                    transpose_store(P, ident, bident, r, br, a, stage, bstage, ptr)
            if x2T is not None:
                P.dma("sp", x2T.rearrange("(k p) t -> p k t", p=128)[:, :, c * 512:(c + 1) * 512],
                      stage[:], reads=[bstage])


def phase_sb_attn(P, QK, V, CAT):
    with P.phase():
        qsb = P.sb([128, 4, T], BF16, "qsb")
        ksb = P.sb([128, 4, T], BF16, "ksb")
        vsb = P.sb([128, NT, 512], BF16, "vsb")
        bq, bk, bv = Buf(), Buf(), Buf()
        QKv = QK.rearrange("(o p) t -> p o t", p=128)
        for o in range(4):
            P.dma("sp", qsb[:, o, :], QKv[:, o, :], writes=[bq])
            P.dma("act", ksb[:, o, :], QKv[:, 4 + o, :], writes=[bk])
        Vv = V.rearrange("(j p) c -> p j c", p=128)
        for j0 in range(0, NT, 8):
            P.dma("sp", vsb[:, j0:j0 + 8, :], Vv[:, j0:j0 + 8, :], writes=[bv])
        U32 = P.sb([128, 128], F32, "U32")
        U = P.sb([128, 128], BF16, "U")
        ones = P.sb([128, 128], BF16, "ones")
        bc = Buf()
        P.op("pool", lambda e: e.memset(U32[:], 1.0), writes=[bc])
        P.op("pool", lambda e: e.affine_select(out=U32[:], in_=U32[:], pattern=[[1, 128]], compare_op=ALU.is_ge,
                                               fill=P.freg(0.0), base=0, channel_multiplier=-1), reads=[bc], writes=[bc])
        P.op("dve", lambda e: e.tensor_copy(out=U[:], in_=U32[:]), reads=[bc], writes=[bc])
        P.op("dve", lambda e: e.memset(ones[:], 1.0), writes=[bc])

        zr = P.psring(3, [128, 512], F32, "z")
        tr = P.psring(1, [128, 512], F32, "tq")
        Rr = P.psring(2, [128, 512], F32, "R")
        Or = P.psring(2, [128, 512], F32, "O")
        er = P.sbring(2, [128, 512], F32, "e")
        spr = P.sbring(6, [128, 512], F32, "sp")
        lr = P.sbring(3, [128, 512], BF16, "L")
        tmr = P.sbring(3, [128, 512], F32, "tmp")
        wr = P.sbring(3, [128, 512], BF16, "w")
        osr = P.sbring(2, [64, 512], BF16, "os")

        units = []
        for hp in range(4):
            for c in range(T // 512):
                grp = [(2 * hp + hh, Rr.next(), Or.next()) for hh in range(2)]
                jmax = 4 * c + 3
                for j in range(jmax, -1, -1):
                    for (h, Rb, Ob) in grp:
                        units.append(dict(h=h, c=c, j=j, jmax=jmax, R=Rb, O=Ob))

        def mask_op(u, tile, btile):
            base = u["c"] * 512 - u["j"] * 128
            P.op("pool", lambda e: e.affine_select(out=tile[:], in_=tile[:], pattern=[[1, 512]],
                                                   compare_op=ALU.is_gt, fill=P.freg(0.0), base=base,
                                                   channel_multiplier=-1), reads=[btile], writes=[btile])

        def st_a(u):
            h, c, j = u["h"], u["c"], u["j"]
            o, pb = h // 2, (h % 2) * 64
            z, bz = zr.next()
            P.op("pe", lambda e: e.matmul(out=z[:], lhsT=ksb[pb:pb + 64, o, j * 128:(j + 1) * 128],
                                          rhs=qsb[pb:pb + 64, o, c * 512:(c + 1) * 512], start=True, stop=True),
                 reads=[bq, bk], writes=[bz])
            u.update(z=z, bz=bz, diag=(j >= 4 * c))

        def st_b(u):
            z, bz = u["z"], u["bz"]
            ee, be = er.next()
            P.op("act", lambda e: e.activation(out=ee[:], in_=z[:], func=AF.Exp, scale=-0.125), reads=[bz], writes=[be])
            sp, bsp = spr.next()
            P.op("act", lambda e: e.activation(out=sp[:], in_=ee[:], func=AF.Ln, bias=1.0, scale=1.0), reads=[be], writes=[bsp])
            u.update(sp=sp, bsp=bsp)

        def st_c(u):
            z, bz, sp, bsp = u["z"], u["bz"], u["sp"], u["bsp"]
            L, bL = lr.next()
            P.op("dve", lambda e: e.scalar_tensor_tensor(out=L[:], in0=z[:], scalar=-0.125, in1=sp[:],
                                                         op0=ALU.mult, op1=ALU.subtract), reads=[bz, bsp], writes=[bL])
            u.update(L=L, bL=bL)
            if u["diag"]:
                mask_op(u, L, bL)

        def st_d(u):
            j = u["j"]
            L, bL = u["L"], u["bL"]
            R, bR = u["R"]
            tq, btq = tr.next()
            P.op("pe", lambda e: e.matmul(out=tq[:], lhsT=U[:], rhs=L[:], start=True, stop=True), reads=[bL, bc], writes=[btq])
            P.op("pe", lambda e: e.matmul(out=R[:], lhsT=ones[:], rhs=L[:], start=(j == u["jmax"]), stop=True),
                 reads=[bL, bc], writes=[bR])
            u.update(tq=tq, btq=btq)

        def st_e(u):
            tq, btq, sp, bsp = u["tq"], u["btq"], u["sp"], u["bsp"]
            R, bR = u["R"]
            tm, btm = tmr.next()
            P.op("dve", lambda e: e.scalar_tensor_tensor(out=tm[:], in0=tq[:], scalar=-1.0, in1=sp[:],
                                                         op0=ALU.mult, op1=ALU.subtract), reads=[btq, bsp], writes=[btm])
            P.op("dve", lambda e: e.tensor_tensor(out=tm[:], in0=tm[:], in1=R[:], op=ALU.add), reads=[btm, bR], writes=[btm])
            u.update(tm=tm, btm=btm)

        def st_f(u):
            tm, btm = u["tm"], u["btm"]
            w, bw = wr.next()
            P.op("act", lambda e: e.activation(out=w[:], in_=tm[:], func=AF.Exp), reads=[btm], writes=[bw])
            u.update(w=w, bw=bw)
            if u["diag"]:
                mask_op(u, w, bw)

        def st_g(u):
            h, c, j = u["h"], u["c"], u["j"]
            O, bO = u["O"]
            w, bw = u["w"], u["bw"]
            P.op("pe", lambda e: e.matmul(out=O[0:64, :], lhsT=vsb[:, j, h * 64:(h + 1) * 64], rhs=w[:],
                                          start=(j == u["jmax"]), stop=(j == 0)), reads=[bw, bv], writes=[bO])
            if j == 0:
                os_, bos = osr.next()
                P.op("act", lambda e: e.copy(out=os_[:], in_=O[0:64, :]), reads=[bO], writes=[bos])
                P.dma("sp", CAT[h * 64:(h + 1) * 64, c * 512:(c + 1) * 512], os_[:], reads=[bos])

        stages = [st_a, st_b, st_c, st_d, st_e, st_f, st_g]
        n = len(units)
        for i in range(n + len(stages) - 1):
            for k in range(len(stages) - 1, -1, -1):
                if 0 <= i - k < n:
                    stages[k](units[i - k])


LC = 256


def phase_ssm(P, UT, prm, Wg, CAT):
    (log_dt, lam_re, lam_im, b_re, b_im, c_re, c_im, dskip, bglu) = prm
    NCH = T // LC
    TW = LC + 1
    TWOPI = 2.0 * math.pi
    with P.phase():
        ident, bident = make_identity(P)
        SINT = P.sb([128, 16, TW], F32, "sint")
        COST = P.sb([128, 16, TW], F32, "cost")
        btab = Buf()
        BDr = P.sb([128, 16, 128], BF16, "BDr")
        BDi = P.sb([128, 16, 128], BF16, "BDi")
        CTr = P.sb([128, 16, 128], BF16, "CTr")
        CTi = P.sb([128, 16, 128], BF16, "CTi")
        bBD, bCT = Buf(), Buf()
        prm_t = P.sb([128, 16, 16], F32, "prm")
        bprm = Buf()
        dcol = P.sb([128, 4], F32, "dcol")
        bgcol = P.sb([128, 4], F32, "bgcol")
        bsm = Buf()
        usb = P.sb([128, 4, T], BF16, "usb")
        ygsb = P.sb([128, 4, T], BF16, "ygsb")
        bu_, byg = Buf(), [Buf() for _ in range(NCH * 4)]
        Wgsb = P.sb([128, 4, 512], BF16, "Wg")
        bWg = Buf()
        pbu = P.psring(4, [128, 512], F32, "pbu")
        py = P.psring(3, [128, 512], F32, "py")

        UTv = UT.rearrange("(q p) t -> p q t", p=128)
        for q in range(4):
            P.dma("sp" if q % 2 else "act", usb[:, q, :], UTv[:, q, :], writes=[bu_])
            P.dma("sp", Wgsb[:, q, :], Wg[q * 128:(q + 1) * 128, :], writes=[bWg])
        def load_T(src2d, R, dst, bdst):
            st = P.sb([128, 128], F32, "ldT")
            b = Buf()
            P.dma("sp", st[0:R, :], src2d, writes=[b])
            pt, bpt = pbu.next()
            P.op("pe", lambda e: e.transpose(out=pt[:, 0:R], in_=st[0:R, :], identity=ident[0:R, 0:R]),
                 reads=[b, bident], writes=[bpt])
            P.op("act", lambda e: e.copy(out=dst, in_=pt[:, 0:R]), reads=[bpt], writes=[bdst])
        load_T(dskip.rearrange("g c -> (g c)").rearrange("(q p) -> q p", p=128), 4, dcol[:], bsm)
        load_T(bglu.rearrange("(q p) -> q p", p=128), 4, bgcol[:], bsm)

        old_es = P.es
        with ExitStack() as st2:
            P.es = st2
            def col(k):
                return prm_t[:, :, k]
            load_T(lam_re.rearrange("(i two) n -> i (two n)", two=2), 16, col(0), bprm)
            load_T(lam_im.rearrange("(i two) n -> i (two n)", two=2), 16, col(1), bprm)
            ld2 = P.sb([16, 2], F32, "ld2")
            ldb = P.sb([16, 128], F32, "ldb")
            bld = Buf()
            P.dma("sp", ld2[:], log_dt.rearrange("(i two) -> i two", two=2), writes=[bld])
            for two in range(2):
                P.op("dve", lambda e, two=two: e.tensor_copy(out=ldb[:, 64 * two:64 * two + 64],
                                                            in_=ld2[:, two:two + 1].to_broadcast([16, 64])),
                     reads=[bld], writes=[bld])
            ptl, bptl = pbu.next()
            P.op("pe", lambda e: e.transpose(out=ptl[:, 0:16], in_=ldb[:], identity=ident[0:16, 0:16]),
                 reads=[bld, bident], writes=[bptl])
            P.op("act", lambda e: e.copy(out=col(2), in_=ptl[:, 0:16]), reads=[bptl], writes=[bprm])

            def vop(fn):
                P.op("dve", fn, reads=[bprm], writes=[bprm])

            def aop(fn):
                P.op("act", fn, reads=[bprm], writes=[bprm])
            aop(lambda e: e.activation(out=col(2), in_=col(2), func=AF.Exp))
            vop(lambda e: e.tensor_tensor(out=col(3), in0=col(0), in1=col(2), op=ALU.mult))
            aop(lambda e: e.activation(out=col(3), in_=col(3), func=AF.Exp))
            vop(lambda e: e.tensor_tensor(out=col(4), in0=col(1), in1=col(2), op=ALU.mult))
            vop(lambda e: e.tensor_scalar(out=col(5), in0=col(4), scalar1=1.0 / TWOPI, scalar2=None, op0=ALU.mult))
            itmp = P.sb([128, 16], I32, "itmp")
            vop(lambda e: e.tensor_copy(out=itmp[:], in_=col(5)))
            vop(lambda e: e.tensor_copy(out=col(13), in_=itmp[:]))
            vop(lambda e: e.tensor_tensor(out=col(6), in0=col(5), in1=col(13), op=ALU.subtract))
            iot = P.sb([128, TW], F32, "iot")
            P.op("pool", lambda e: e.iota(out=iot[:], pattern=[[1, TW]], base=0, channel_multiplier=0,
                                          allow_small_or_imprecise_dtypes=True), writes=[bprm])
            angr = P.sbring(2, [128, TW], F32, "ang")
            angi = P.sbring(2, [128, TW], I32, "angi")
            angc = P.sbring(2, [128, TW], F32, "angc")
            SC = TWOPI * (1.0 - 2e-6)
            for i in range(16):
                a, ba = angr.next()
                ai, bai = angi.next()
                ac, bac = angc.next()
                P.op("dve", lambda e, a=a, i=i: e.tensor_scalar(out=a[:], in0=iot[:], scalar1=prm_t[:, i, 6:7], scalar2=None,
                                                                op0=ALU.mult), reads=[bprm], writes=[ba])
                P.op("dve", lambda e, a=a, ai=ai: e.tensor_copy(out=ai[:], in_=a[:]), reads=[ba], writes=[bai])
                P.op("dve", lambda e, ac=ac, ai=ai: e.tensor_copy(out=ac[:], in_=ai[:]), reads=[bai], writes=[bac])
                P.op("dve", lambda e, a=a, ac=ac: e.tensor_tensor(out=a[:], in0=a[:], in1=ac[:], op=ALU.subtract),
                     reads=[ba, bac], writes=[ba])
                P.op("act", lambda e, a=a, i=i: e.activation(out=SINT[:, i, :], in_=a[:], func=AF.Sin, scale=SC),
                     reads=[ba], writes=[btab])
                P.op("dve", lambda e, a=a, ac=ac: e.tensor_scalar(out=ac[:], in0=a[:], scalar1=0.25, scalar2=0.5,
                                                                  op0=ALU.add, op1=ALU.is_gt), reads=[ba, bac], writes=[bac])
                P.op("dve", lambda e, a=a, ac=ac: e.scalar_tensor_tensor(out=a[:], in0=a[:], scalar=0.25, in1=ac[:],
                                                                         op0=ALU.add, op1=ALU.subtract),
                     reads=[ba, bac], writes=[ba])
                P.op("act", lambda e, a=a, i=i: e.activation(out=COST[:, i, :], in_=a[:], func=AF.Sin, scale=SC),
                     reads=[ba], writes=[btab])
            P.op("dve", lambda e: e.tensor_tensor(out=col(7), in0=col(3), in1=COST[:, :, 1], op=ALU.mult), reads=[bprm, btab], writes=[bprm])
            P.op("dve", lambda e: e.tensor_tensor(out=col(8), in0=col(3), in1=SINT[:, :, 1], op=ALU.mult), reads=[bprm, btab], writes=[bprm])
            vop(lambda e: e.tensor_tensor(out=col(9), in0=col(0), in1=col(0), op=ALU.mult))
            vop(lambda e: e.tensor_tensor(out=col(13), in0=col(1), in1=col(1), op=ALU.mult))
            vop(lambda e: e.tensor_tensor(out=col(9), in0=col(9), in1=col(13), op=ALU.add))
            vop(lambda e: e.reciprocal(out=col(9), in_=col(9)))
            vop(lambda e: e.tensor_scalar(out=col(10), in0=col(7), scalar1=-1.0, scalar2=None, op0=ALU.add))
            vop(lambda e: e.tensor_tensor(out=col(13), in0=col(10), in1=col(0), op=ALU.mult))
            vop(lambda e: e.tensor_tensor(out=col(14), in0=col(8), in1=col(1), op=ALU.mult))
            vop(lambda e: e.tensor_tensor(out=col(13), in0=col(13), in1=col(14), op=ALU.add))
            vop(lambda e: e.tensor_tensor(out=col(11), in0=col(13), in1=col(9), op=ALU.mult))
            vop(lambda e: e.tensor_tensor(out=col(13), in0=col(8), in1=col(0), op=ALU.mult))
            vop(lambda e: e.tensor_tensor(out=col(14), in0=col(10), in1=col(1), op=ALU.mult))
            vop(lambda e: e.tensor_tensor(out=col(13), in0=col(13), in1=col(14), op=ALU.subtract))
            vop(lambda e: e.tensor_tensor(out=col(12), in0=col(13), in1=col(9), op=ALU.mult))

            XBr = P.sb([128, 16, 128], F32, "XBr")
            XBi = P.sb([128, 16, 128], F32, "XBi")
            Bsr = P.sb([128, 16, 16], F32, "Bsr")
            Bsi = P.sb([128, 16, 16], F32, "Bsi")
            Cnr = P.sb([128, 4, 64], F32, "Cnr")
            Cni = P.sb([128, 4, 64], F32, "Cni")
            bX, bBs, bCn = Buf(), Buf(), Buf()
            P.op("dve", lambda e: e.memset(XBr[:], 0.0), writes=[bX])
            P.op("pool", lambda e: e.memset(XBi[:], 0.0), writes=[bX])
            P.op("dve", lambda e: e.memset(CTr[:], 0.0), writes=[bCT])
            P.op("pool", lambda e: e.memset(CTi[:], 0.0), writes=[bCT])
            P.dma("sp", Bsr[:], b_re.rearrange("g n c -> (g n) c").rearrange("(i p) c -> p i c", p=128), writes=[bBs])
            P.dma("act", Bsi[:], b_im.rearrange("g n c -> (g n) c").rearrange("(i p) c -> p i c", p=128), writes=[bBs])
            P.dma("sp", Cnr[:], c_re.rearrange("g c n -> (g c) n").rearrange("(q p) n -> p q n", p=128), writes=[bCn])
            P.dma("act", Cni[:], c_im.rearrange("g c n -> (g c) n").rearrange("(q p) n -> p q n", p=128), writes=[bCn])
            for (X, Bs) in ((XBr, Bsr), (XBi, Bsi)):
                X4 = X[:].rearrange("p (q r) c -> p q r c", r=4)
                B4 = Bs[:].rearrange("p (q r) c -> p q r c", r=4)
                for r in range(4):
                    for two in range(2):
                        c0 = 32 * r + 16 * two
                        o_ = X4[64 * two:64 * two + 64, :, r, c0:c0 + 16]
                        i_ = B4[64 * two:64 * two + 64, :, r, :]
                        P.op("dve", lambda e, o_=o_, i_=i_: e.tensor_copy(out=o_, in_=i_), reads=[bBs, bX], writes=[bX])
            tmpr = P.sbring(2, [128, 128], F32, "tmpx")
            bbr = P.sbring(2, [128, 128], F32, "bbx")
            for i in range(16):
                cr_, ci_ = prm_t[:, i, 11:12], prm_t[:, i, 12:13]
                for which in range(2):
                    tm, btm = tmpr.next()
                    bb, bbb = bbr.next()
                    if which == 0:
                        P.op("dve", lambda e, tm=tm, i=i, ci_=ci_: e.tensor_scalar(out=tm[:], in0=XBi[:, i, :], scalar1=ci_, scalar2=None, op0=ALU.mult),
                             reads=[bX, bprm], writes=[btm])
                        P.op("dve", lambda e, tm=tm, bb=bb, i=i, cr_=cr_: e.scalar_tensor_tensor(out=bb[:], in0=XBr[:, i, :], scalar=cr_, in1=tm[:],
                                                                                         op0=ALU.mult, op1=ALU.subtract),
                             reads=[bX, bprm, btm], writes=[bbb])
                    else:
                        P.op("dve", lambda e, tm=tm, i=i, ci_=ci_: e.tensor_scalar(out=tm[:], in0=XBr[:, i, :], scalar1=ci_, scalar2=None, op0=ALU.mult),
                             reads=[bX, bprm], writes=[btm])
                        P.op("dve", lambda e, tm=tm, bb=bb, i=i, cr_=cr_: e.scalar_tensor_tensor(out=bb[:], in0=XBi[:, i, :], scalar=cr_, in1=tm[:],
                                                                                         op0=ALU.mult, op1=ALU.add),
                             reads=[bX, bprm, btm], writes=[bbb])
                    pt, bpt = pbu.next()
                    P.op("pe", lambda e, pt=pt, bb=bb: e.transpose(out=pt[:, 0:128], in_=bb[:], identity=ident[:]),
                         reads=[bbb, bident], writes=[bpt])
                    dst = (BDr if which == 0 else BDi)[:, i, :]
                    P.op("act", lambda e, dst=dst, pt=pt: e.copy(out=dst, in_=pt[:, 0:128]), reads=[bpt], writes=[bBD])
            for which, (Cn, CT) in enumerate(((Cnr, CTr), (Cni, CTi))):
                sgn = 1.0 if which == 0 else -1.0
                for qp in range(4):
                    pt, bpt = pbu.next()
                    P.op("pe", lambda e, pt=pt, Cn=Cn, qp=qp: e.transpose(out=pt[0:64, 0:128], in_=Cn[:, qp, :], identity=ident[:]),
                         reads=[bCn, bident], writes=[bpt])
                    for r in range(4):
                        i = 4 * qp + r
                        for two in range(2):
                            c0 = 32 * r + 16 * two
                            P.op("act", lambda e, pt=pt, CT=CT, i=i, two=two, c0=c0, sgn=sgn: e.mul(
                                out=CT[64 * two:64 * two + 64, i, c0:c0 + 16], in_=pt[0:64, c0:c0 + 16], mul=sgn),
                                reads=[bpt, bCT], writes=[bCT])
            P.barrier()
        P.es = old_es

        R2 = lambda n, dt=F32, nm="r": P.sbring(n, [128, LC], dt, nm)
        bur_s, bui_s = R2(2, F32, "burs"), R2(2, F32, "buis")
        t1r, t2r, t3r, t4r = R2(2), R2(2), R2(2), R2(2)
        bpr, bpi = R2(2, F32, "bpr"), R2(2, F32, "bpi")
        xpr, xpi = R2(2, F32, "xpr"), R2(2, F32, "xpi")
        u1r, u2r, u3r, u4r = R2(2), R2(2), R2(2), R2(2)
        xrr, xir = R2(6, BF16, "xr"), R2(6, BF16, "xi")
        yr_, sqr_, p1r, p2r, sgr = R2(2), R2(2), R2(2), R2(2), R2(2)
        zsg = P.sbring(2, [128, LC], F32, "zsg")
        gor = P.sbring(3, [128, LC], BF16, "go")
        inits = [[(P.sb([128, 4], F32, "init"), Buf()) for _ in range(2)] for _ in range(16)]
        for i in range(16):
            P.op("dve", lambda e, i=i: e.memset(inits[i][0][0][:], 0.0), writes=[inits[i][0][1]])

        for k in range(NCH):
            tsl = slice(k * LC, (k + 1) * LC)
            for q in range(4):
                xs = []
                for r in range(4):
                    i = 4 * q + r
                    cosT, sinT = COST[:, i, 0:LC], SINT[:, i, 0:LC]
                    pr_, bpr_ = pbu.next()
                    pi_, bpi_ = pbu.next()
                    P.op("pe", lambda e, pr_=pr_, i=i: e.matmul(out=pr_[:, :LC], lhsT=BDr[:, i, :], rhs=usb[:, q, tsl], start=True, stop=True),
                         reads=[bBD, bu_], writes=[bpr_])
                    P.op("pe", lambda e, pi_=pi_, i=i: e.matmul(out=pi_[:, :LC], lhsT=BDi[:, i, :], rhs=usb[:, q, tsl], start=True, stop=True),
                         reads=[bBD, bu_], writes=[bpi_])
                    brs, bbrs = bur_s.next()
                    bis, bbis = bui_s.next()
                    P.op("act", lambda e, brs=brs, pr_=pr_: e.copy(out=brs[:], in_=pr_[:, :LC]), reads=[bpr_], writes=[bbrs])
                    P.op("act", lambda e, bis=bis, pi_=pi_: e.copy(out=bis[:], in_=pi_[:, :LC]), reads=[bpi_], writes=[bbis])
                    t1, b1 = t1r.next(); t2, b2 = t2r.next(); t3, b3 = t3r.next(); t4, b4 = t4r.next()
                    P.op("dve", lambda e, t1=t1, pr_=pr_, cosT=cosT: e.tensor_tensor(out=t1[:], in0=pr_[:, :LC], in1=cosT, op=ALU.mult), reads=[bpr_, btab], writes=[b1])
                    P.op("pool", lambda e, t2=t2, bis=bis, sinT=sinT: e.tensor_tensor(out=t2[:], in0=bis[:], in1=sinT, op=ALU.mult), reads=[bbis, btab], writes=[b2])
                    P.op("dve", lambda e, t3=t3, pi_=pi_, cosT=cosT: e.tensor_tensor(out=t3[:], in0=pi_[:, :LC], in1=cosT, op=ALU.mult), reads=[bpi_, btab], writes=[b3])
                    P.op("pool", lambda e, t4=t4, brs=brs, sinT=sinT: e.tensor_tensor(out=t4[:], in0=brs[:], in1=sinT, op=ALU.mult), reads=[bbrs, btab], writes=[b4])
                    br_, bbr_ = bpr.next(); bi_, bbi_ = bpi.next()
                    P.op("dve", lambda e, br_=br_, t1=t1, t2=t2: e.tensor_tensor(out=br_[:], in0=t1[:], in1=t2[:], op=ALU.add), reads=[b1, b2], writes=[bbr_])
                    P.op("pool", lambda e, bi_=bi_, t3=t3, t4=t4: e.tensor_tensor(out=bi_[:], in0=t3[:], in1=t4[:], op=ALU.subtract), reads=[b3, b4], writes=[bbi_])
                    ini, bini = inits[i][k % 2]
                    nin, bnin = inits[i][(k + 1) % 2]
                    mbc = prm_t[:, i, 3:4].to_broadcast([128, LC])
                    xr_, bxr_ = xpr.next(); xi_, bxi_ = xpi.next()
                    P.op("dve", lambda e, xr_=xr_, br_=br_, ini=ini, mbc=mbc: e.tensor_tensor_scan(out=xr_[:], data0=mbc, data1=br_[:], initial=ini[:, 0:1],
                                                                                          op0=ALU.mult, op1=ALU.add), reads=[bbr_, bini, bprm], writes=[bxr_])
                    P.op("dve", lambda e, xi_=xi_, bi_=bi_, ini=ini, mbc=mbc: e.tensor_tensor_scan(out=xi_[:], data0=mbc, data1=bi_[:], initial=ini[:, 1:2],
                                                                                          op0=ALU.mult, op1=ALU.add), reads=[bbi_, bini, bprm], writes=[bxi_])
                    cL, sL = COST[:, i, LC:LC + 1], SINT[:, i, LC:LC + 1]
                    lr_, li_ = xr_[:, LC - 1:LC], xi_[:, LC - 1:LC]
                    P.op("dve", lambda e, nin=nin, li_=li_, sL=sL: e.tensor_scalar(out=nin[:, 2:3], in0=li_, scalar1=sL, scalar2=None, op0=ALU.mult),
                         reads=[bxi_, btab], writes=[bnin])
                    P.op("dve", lambda e, nin=nin, lr_=lr_, cL=cL: e.scalar_tensor_tensor(out=nin[:, 0:1], in0=lr_, scalar=cL, in1=nin[:, 2:3], op0=ALU.mult, op1=ALU.subtract),
                         reads=[bxr_, btab, bnin], writes=[bnin])
                    P.op("dve", lambda e, nin=nin, li_=li_, cL=cL: e.tensor_scalar(out=nin[:, 3:4], in0=li_, scalar1=cL, scalar2=None, op0=ALU.mult),
                         reads=[bxi_, btab, bnin], writes=[bnin])
                    P.op("dve", lambda e, nin=nin, lr_=lr_, sL=sL: e.scalar_tensor_tensor(out=nin[:, 1:2], in0=lr_, scalar=sL, in1=nin[:, 3:4], op0=ALU.mult, op1=ALU.add),
                         reads=[bxr_, btab, bnin], writes=[bnin])
                    u1, c1 = u1r.next(); u2, c2 = u2r.next(); u3, c3 = u3r.next(); u4, c4 = u4r.next()
                    P.op("dve", lambda e, u1=u1, xr_=xr_, cosT=cosT: e.tensor_tensor(out=u1[:], in0=xr_[:], in1=cosT, op=ALU.mult), reads=[bxr_, btab], writes=[c1])
                    P.op("pool", lambda e, u2=u2, xi_=xi_, sinT=sinT: e.tensor_tensor(out=u2[:], in0=xi_[:], in1=sinT, op=ALU.mult), reads=[bxi_, btab], writes=[c2])
                    P.op("pool", lambda e, u3=u3, xr_=xr_, sinT=sinT: e.tensor_tensor(out=u3[:], in0=xr_[:], in1=sinT, op=ALU.mult), reads=[bxr_, btab], writes=[c3])
                    P.op("pool", lambda e, u4=u4, xi_=xi_, cosT=cosT: e.tensor_tensor(out=u4[:], in0=xi_[:], in1=cosT, op=ALU.mult), reads=[bxi_, btab], writes=[c4])
                    xr, bxr = xrr.next(); xi, bxi = xir.next()
                    P.op("dve", lambda e, xr=xr, u1=u1, u2=u2: e.tensor_tensor(out=xr[:], in0=u1[:], in1=u2[:], op=ALU.subtract), reads=[c1, c2], writes=[bxr])
                    P.op("pool", lambda e, xi=xi, u3=u3, u4=u4: e.tensor_tensor(out=xi[:], in0=u3[:], in1=u4[:], op=ALU.add), reads=[c3, c4], writes=[bxi])
                    xs.append((i, xr, bxr, xi, bxi))
                yp, byp = py.next()
                for n_, (i, xr, bxr, xi, bxi) in enumerate(xs):
                    P.op("pe", lambda e, yp=yp, i=i, xr=xr, n_=n_: e.matmul(out=yp[:, :LC], lhsT=CTr[:, i, :], rhs=xr[:], start=(n_ == 0), stop=False),
                         reads=[bCT, bxr], writes=[byp])
                    P.op("pe", lambda e, yp=yp, i=i, xi=xi, n_=n_: e.matmul(out=yp[:, :LC], lhsT=CTi[:, i, :], rhs=xi[:], start=False, stop=(n_ == 3)),
                         reads=[bCT, bxi], writes=[byp])
                y, by = yr_.next()
                P.op("dve", lambda e, y=y, yp=yp: e.scalar_tensor_tensor(out=y[:], in0=usb[:, q, tsl], scalar=dcol[:, q:q + 1], in1=yp[:, :LC],
                                                                       op0=ALU.mult, op1=ALU.add), reads=[bu_, bsm, byp], writes=[by])
                s2, bs2 = sqr_.next(); p1, bp1 = p1r.next(); p2, bp2 = p2r.next(); sg, bsg = sgr.next()
                P.op("act", lambda e, s2=s2, y=y: e.activation(out=s2[:], in_=y[:], func=AF.Square), reads=[by], writes=[bs2])
                P.op("dve", lambda e, p1=p1, s2=s2: e.tensor_scalar(out=p1[:], in0=s2[:], scalar1=0.044715, scalar2=1.0, op0=ALU.mult, op1=ALU.add),
                     reads=[bs2], writes=[bp1])
                P.op("pool", lambda e, p2=p2, p1=p1, y=y: e.tensor_tensor(out=p2[:], in0=p1[:], in1=y[:], op=ALU.mult), reads=[bp1, by], writes=[bp2])
                P.op("act", lambda e, sg=sg, p2=p2: e.activation(out=sg[:], in_=p2[:], func=AF.Sigmoid, scale=1.5957691216057308), reads=[bp2], writes=[bsg])
                P.op("dve", lambda e, sg=sg, y=y: e.tensor_tensor(out=ygsb[:, q, tsl], in0=y[:], in1=sg[:], op=ALU.mult), reads=[by, bsg], writes=[byg[k * 4 + q]])
            for o in range(4):
                zp, bzp = py.next()
                for qq in range(4):
                    P.op("pe", lambda e, zp=zp, qq=qq, o=o: e.matmul(out=zp[:, :LC], lhsT=Wgsb[:, qq, o * 128:(o + 1) * 128], rhs=ygsb[:, qq, tsl],
                                                                  start=(qq == 0), stop=(qq == 3)), reads=[bWg, byg[k * 4 + qq]], writes=[bzp])
                zs, bzs = zsg.next()
                P.op("act", lambda e, zs=zs, zp=zp, o=o: e.activation(out=zs[:], in_=zp[:, :LC], func=AF.Sigmoid, bias=bgcol[:, o:o + 1], scale=1.0),
                     reads=[bzp, bsm], writes=[bzs])
                go, bgo = gor.next()
                P.op("dve", lambda e, go=go, zs=zs, o=o: e.tensor_tensor(out=go[:], in0=ygsb[:, o, tsl], in1=zs[:], op=ALU.mult),
                     reads=[byg[k * 4 + o], bzs], writes=[bgo])
                P.dma("sp", CAT[512 + o * 128:512 + (o + 1) * 128, tsl], go[:], reads=[bgo])


def phase_dsa(P, FM, Vd, WI, CAT):
    NQ = T // 512
    with P.phase():
        ident, bident = make_identity(P)
        identb = P.sb([128, 128], BF16, "identb")
        bidb = Buf()
        P.op("dve", lambda e: e.tensor_copy(out=identb[:], in_=ident[:]), reads=[bident], writes=[bidb])
        ones32 = P.sb([128, 128], F32, "ones32")
        sel = P.sb([128, 64], F32, "sel")
        bcst = Buf()
        P.op("dve", lambda e: e.memset(ones32[:], 1.0), writes=[bcst])
        P.op("dve", lambda e: e.memset(sel[:], 0.0), writes=[bcst])
        P.op("dve", lambda e: e.memset(sel[64:65, :], 1.0), reads=[bcst], writes=[bcst])
        D0 = P.sb([128, 512], F32, "D0")
        P.op("pool", lambda e: e.iota(out=D0[:], pattern=[[1, 512]], base=0, channel_multiplier=-1,
                                      allow_small_or_imprecise_dtypes=True), writes=[bcst])
        iotaS = P.sb([128, T], F32, "iotaS")
        P.op("pool", lambda e: e.iota(out=iotaS[:], pattern=[[1, T]], base=1, channel_multiplier=0,
                                      allow_small_or_imprecise_dtypes=True), writes=[bcst])
        tcol = P.sb([128, NT], F32, "tcol")
        P.op("pool", lambda e: e.iota(out=tcol[:], pattern=[[128, NT]], base=1, channel_multiplier=1,
                                      allow_small_or_imprecise_dtypes=True), writes=[bcst])

        kisb = P.sb([128, T], BF16, "kisb")
        ksb = P.sb([128, 2, T], BF16, "ksb")
        wisb = P.sb([128, NT, 8], F32, "wisb")
        vsb = P.sb([128, NT, 4, 65], BF16, "vsb")
        bki, bk, bwi, bv = Buf(), Buf(), Buf(), Buf()
        P.dma("sp", kisb[:], FM[1920:2048, :], writes=[bki])
        for o in range(2):
            P.dma("act", ksb[:, o, :], FM[1024 + 128 * o:1024 + 128 * (o + 1), :], writes=[bk])
        P.dma("sp", wisb[:], WI.rearrange("(i p) h -> p i h", p=128), writes=[bwi])
        P.op("pool", lambda e: e.memset(vsb[:], 1.0), writes=[bv])
        Vv = Vd.rearrange("(j p) (g d) -> p j g d", p=128, g=4)
        for j0 in range(0, NT, 8):
            for g in range(4):
                P.dma("sp", vsb[:, j0:j0 + 8, g, 0:64], Vv[:, j0:j0 + 8, g, :], writes=[bv])

        acc = P.sb([128, T], F32, "acc")
        bacc = Buf()
        maskbf = P.sb([128, T], BF16, "maskbf")
        bmask = Buf()
        maskT = P.sb([128, NT, 512], BF16, "maskT")
        bmT = [Buf() for _ in range(4)]
        Dm = P.sb([128, 512], F32, "Dm")
        bDm = Buf()
        sm = P.sbring(2, [128, 16], F32, "sm")
        qir = P.sbring(2, [128, 3, 128], BF16, "qi")
        qcr = P.sbring(2, [128, 8, 512], BF16, "qc")
        rr = P.sbring(3, [128, 512], F32, "relu")
        tmr = P.sbring(3, [128, 512], F32, "tmp")
        pr = P.sbring(5, [128, 512], BF16, "p")
        dcr = P.sbring(2, [128, 512], F32, "dcj")
        dgr = P.sbring(2, [128, 128], F32, "dg")
        osr = P.sbring(2, [128, 512], F32, "osb")
        recr = P.sbring(2, [64, 512], F32, "rec")
        outr = P.sbring(2, [64, 512], BF16, "outb")
        pzi = P.psring(1, [128, 512], F32, "pzi")
        ptm = P.psring(1, [128, 512], BF16, "ptm")
        pz = P.psring(2, [128, 512], F32, "pz")
        po = P.psring(4, [128, 512], F32, "po")
        FMq = FM[0:1024, :].rearrange("(o p) t -> p o t", p=128)
        FMqi = FM[1536:1920, :].rearrange("(o p) t -> p o t", p=128)

        for c in range(NQ):
            for a in range(4):
                i = 4 * c + a
                S = 128 * (i + 1)
                qi, bqi = qir.next()
                P.dma("sp", qi[:], FMqi[:, :, i * 128:(i + 1) * 128], writes=[bqi])
                for h in range(8):
                    pb, ot = 32 * (h % 3), h // 3
                    for s0 in range(0, S, 512):
                        sn = min(512, S - s0)
                        z, bz = pz.next()
                        P.op("pe", lambda e, z=z, qi=qi, pb=pb, ot=ot, s0=s0, sn=sn: e.matmul(
                            out=z[:, :sn], lhsT=qi[pb:pb + 32, ot, :], rhs=kisb[pb:pb + 32, s0:s0 + sn],
                            start=True, stop=True), reads=[bqi, bki], writes=[bz])
                        r, br = rr.next()
                        P.op("act", lambda e, r=r, z=z, sn=sn: e.activation(out=r[:, :sn], in_=z[:, :sn], func=AF.Relu),
                             reads=[bz], writes=[br])
                        if h == 0:
                            P.op("dve", lambda e, r=r, s0=s0, sn=sn, i=i, h=h: e.tensor_scalar(
                                out=acc[:, s0:s0 + sn], in0=r[:, :sn], scalar1=wisb[:, i, h:h + 1], scalar2=None,
                                op0=ALU.mult), reads=[br, bwi], writes=[bacc])
                        else:
                            P.op("dve", lambda e, r=r, s0=s0, sn=sn, i=i, h=h: e.scalar_tensor_tensor(
                                out=acc[:, s0:s0 + sn], in0=r[:, :sn], scalar=wisb[:, i, h:h + 1], in1=acc[:, s0:s0 + sn],
                                op0=ALU.mult, op1=ALU.add), reads=[br, bwi, bacc], writes=[bacc])
                P.op("pool", lambda e, i=i: e.affine_select(out=acc[:, i * 128:(i + 1) * 128], in_=acc[:, i * 128:(i + 1) * 128],
                                                           pattern=[[-1, 128]], compare_op=ALU.is_ge, fill=P.freg(-BIG), base=0,
                                                           channel_multiplier=1), reads=[bacc], writes=[bacc])
                s_, bs_ = sm.next()
                if i >= 2:
                    P.op("dve", lambda e, s_=s_, S=S: e.tensor_reduce(out=s_[:, 1:2], in_=acc[:, 0:S], axis=AX.X, op=ALU.max),
                         reads=[bacc], writes=[bs_])
                    P.op("dve", lambda e, s_=s_, i=i: e.tensor_reduce(out=s_[:, 0:1], in_=acc[:, 0:128 * i], axis=AX.X, op=ALU.min),
                         reads=[bacc, bs_], writes=[bs_])
                    P.op("dve", lambda e, s_=s_: e.tensor_tensor(out=s_[:, 5:6], in0=s_[:, 1:2], in1=s_[:, 0:1], op=ALU.subtract),
                         reads=[bs_], writes=[bs_])
                    for it in range(NBISECT):
                        hw = 2.0 ** (-(it + 1))
                        P.op("dve", lambda e, s_=s_, hw=hw: e.scalar_tensor_tensor(out=s_[:, 2:3], in0=s_[:, 5:6], scalar=hw, in1=s_[:, 0:1],
                                                                                 op0=ALU.mult, op1=ALU.add), reads=[bs_], writes=[bs_])
                        P.op("dve", lambda e, s_=s_, S=S: e.tensor_scalar(out=maskbf[:, 0:S], in0=acc[:, 0:S], scalar1=s_[:, 2:3], scalar2=0.0,
                                                                        op0=ALU.is_ge, op1=ALU.add, accum_out=s_[:, 3:4]),
                             reads=[bacc, bs_, bmask], writes=[bmask, bs_])
                        P.op("dve", lambda e, s_=s_: e.scalar_tensor_tensor(out=s_[:, 4:5], in0=s_[:, 3:4], scalar=float(TOPK), in1=s_[:, 5:6],
                                                                            op0=ALU.is_ge, op1=ALU.mult), reads=[bs_], writes=[bs_])
                        P.op("dve", lambda e, s_=s_, hw=hw: e.scalar_tensor_tensor(out=s_[:, 0:1], in0=s_[:, 4:5], scalar=hw, in1=s_[:, 0:1],
                                                                                 op0=ALU.mult, op1=ALU.add), reads=[bs_], writes=[bs_])
                    P.op("dve", lambda e, s_=s_, S=S: e.tensor_scalar(out=maskbf[:, 0:S], in0=acc[:, 0:S], scalar1=s_[:, 0:1], scalar2=None,
                                                                    op0=ALU.is_lt), reads=[bacc, bs_, bmask], writes=[bmask])
                else:
                    P.op("dve", lambda e, S=S: e.tensor_scalar(out=maskbf[:, 0:S], in0=acc[:, 0:S], scalar1=-0.5 * BIG, scalar2=None,
                                                             op0=ALU.is_lt), reads=[bacc, bmask], writes=[bmask])
                P.op("dve", lambda e, S=S: e.scalar_tensor_tensor(out=acc[:, 0:S], in0=maskbf[:, 0:S], scalar=-float(T + 1), in1=iotaS[:, 0:S],
                                                                 op0=ALU.mult, op1=ALU.add), reads=[bmask, bcst, bacc], writes=[bacc])
                P.op("dve", lambda e, s_=s_, S=S: e.tensor_reduce(out=s_[:, 7:8], in_=acc[:, 0:S], axis=AX.X, op=ALU.max),
                     reads=[bacc, bs_], writes=[bs_])
                P.op("dve", lambda e, s_=s_, i=i: e.tensor_tensor(out=s_[:, 8:9], in0=tcol[:, i:i + 1], in1=s_[:, 7:8], op=ALU.subtract),
                     reads=[bs_, bcst], writes=[bs_])
                dg, bdg = dgr.next()
                P.op("dve", lambda e, dg=dg, s_=s_: e.tensor_scalar(out=dg[:], in0=ident[:], scalar1=s_[:, 8:9], scalar2=None, op0=ALU.mult),
                     reads=[bident, bs_], writes=[bdg])
                zb, bzb = pzi.next()
                P.op("pe", lambda e, zb=zb, dg=dg: e.matmul(out=zb[:, 0:128], lhsT=ones32[:], rhs=dg[:], start=True, stop=True),
                     reads=[bcst, bdg], writes=[bzb])
                P.op("dve", lambda e, zb=zb, a=a: e.tensor_tensor(out=Dm[:, a * 128:(a + 1) * 128], in0=D0[:, a * 128:(a + 1) * 128],
                                                                 in1=zb[:, 0:128], op=ALU.subtract), reads=[bzb, bcst, bDm], writes=[bDm])
                for j0 in range(0, i + 1, 4):
                    jn = min(4, i + 1 - j0)
                    pt, bpt = ptm.next()
                    for jj in range(jn):
                        j = j0 + jj
                        P.op("pe", lambda e, pt=pt, jj=jj, j=j: e.transpose(out=pt[:, jj * 128:(jj + 1) * 128],
                                                                           in_=maskbf[:, j * 128:(j + 1) * 128], identity=identb[:]),
                             reads=[bmask, bidb], writes=[bpt])
                    o_ = maskT[:, j0:j0 + jn, a * 128:(a + 1) * 128]
                    i_ = pt[:, 0:jn * 128].rearrange("p (k t) -> p k t", k=jn)
                    P.op("act", lambda e, o_=o_, i_=i_: e.copy(out=o_, in_=i_), reads=[bpt], writes=[bmT[a]])
                if i + 1 <= 4 * c + 3:
                    P.op("pool", lambda e, i=i, a=a, c=c: e.memset(maskT[:, i + 1:4 * c + 4, a * 128:(a + 1) * 128], 1.0), writes=[bmT[a]])

            qc, bqc = qcr.next()
            P.dma("sp", qc[:], FMq[:, :, c * 512:(c + 1) * 512], writes=[bqc])
            jn_all = 4 * c + 4
            units = []
            for g in range(4):
                Obs = [po.next() for _ in range(4)]
                for j in range(jn_all):
                    for r in range(4):
                        units.append(dict(g=g, r=r, j=j, O=Obs[r]))
            dcj_cache = {}

            def get_dcj(j):
                if j in dcj_cache:
                    return dcj_cache[j]
                d, bd = dcr.next()
                off = float(512 * c - 128 * j)
                P.op("dve", lambda e, d=d, off=off: e.tensor_scalar(out=d[:], in0=Dm[:], scalar1=off, scalar2=0.0, op0=ALU.add, op1=ALU.max),
                     reads=[bDm], writes=[bd])
                P.op("dve", lambda e, d=d, j=j: e.scalar_tensor_tensor(out=d[:], in0=maskT[:, j, :], scalar=MASKBIG, in1=d[:],
                                                                      op0=ALU.mult, op1=ALU.add), reads=[bd] + bmT, writes=[bd])
                dcj_cache.clear()
                dcj_cache[j] = (d, bd)
                return d, bd

            def B0(u):
                g, r, j = u["g"], u["r"], u["j"]
                pb, kt, qt = 64 * (g % 2), g // 2, 4 * (g // 2) + r
                z, bz = pz.next()
                P.op("pe", lambda e: e.matmul(out=z[:], lhsT=ksb[pb:pb + 64, kt, j * 128:(j + 1) * 128], rhs=qc[pb:pb + 64, qt, :],
                                              start=True, stop=True), reads=[bk, bqc], writes=[bz])
                u.update(z=z, bz=bz)

            def B1(u):
                g, r, j = u["g"], u["r"], u["j"]
                h = 4 * g + r
                slope = 2.0 ** (-8.0 * (h + 1) / 16.0)
                z, bz = u["z"], u["bz"]
                d, bd = get_dcj(j)
                tm, btm = tmr.next()
                P.op("dve", lambda e: e.scalar_tensor_tensor(out=tm[:], in0=d[:], scalar=-slope, in1=z[:], op0=ALU.mult, op1=ALU.add),
                     reads=[bd, bz], writes=[btm])
                u.update(tm=tm, btm=btm)

            def B2(u):
                tm, btm = u["tm"], u["btm"]
                p, bp = pr.next()
                P.op("act", lambda e: e.activation(out=p[:], in_=tm[:], func=AF.Exp), reads=[btm], writes=[bp])
                u.update(pm=p, bpm=bp)

            def B3(u):
                g, r, j = u["g"], u["r"], u["j"]
                h = 4 * g + r
                O, bO = u["O"]
                pm, bpm = u["pm"], u["bpm"]
                P.op("pe", lambda e: e.matmul(out=O[0:65, :], lhsT=vsb[:, j, g, :], rhs=pm[:], start=(j == 0), stop=(j == jn_all - 1)),
                     reads=[bpm, bv], writes=[bO])
                if j == jn_all - 1:
                    osb, bos = osr.next()
                    P.op("act", lambda e: e.copy(out=osb[0:65, :], in_=O[0:65, :]), reads=[bO], writes=[bos])
                    dn, bdn = pzi.next()
                    P.op("pe", lambda e: e.matmul(out=dn[0:64, :], lhsT=sel[0:65, :], rhs=osb[0:65, :], start=True, stop=True),
                         reads=[bos, bcst], writes=[bdn])
                    rec, brec = recr.next()
                    P.op("dve", lambda e: e.reciprocal(out=rec[:], in_=dn[0:64, :]), reads=[bdn], writes=[brec])
                    ob, bob = outr.next()
                    P.op("pool", lambda e: e.tensor_tensor(out=ob[:], in0=osb[0:64, :], in1=rec[:], op=ALU.mult), reads=[bos, brec], writes=[bob])
                    P.dma("sp", CAT[h * 64:(h + 1) * 64, c * 512:(c + 1) * 512], ob[:], reads=[bob])

            stages = [B0, B1, B2, B3]
            n = len(units)
            for idx in range(n + len(stages) - 1):
                for k in range(len(stages) - 1, -1, -1):
                    if 0 <= idx - k < n:
                        stages[k](units[idx - k])


IN_SHAPES = {
    "x": [T, D], "sb_ssm_w_in": [2, D, 2048], "ssm_log_dt": [2, 32], "ssm_lam_re": [2, 32, 64], "ssm_lam_im": [2, 32, 64],
    "ssm_b_re": [2, 32, 64, 16], "ssm_b_im": [2, 32, 64, 16], "ssm_c_re": [2, 32, 16, 64], "ssm_c_im": [2, 32, 16, 64],
    "ssm_d": [2, 32, 16], "ssm_w_glu": [2, 512, 512], "ssm_b_glu": [2, 512], "sb_ssm_w_out": [2, D, D],
    "dsa_w_in": [2, D, 1832], "dsa_w_out": [2, D, D], "ln_mix_g": [4, D], "ln_mix_b": [4, D], "ln_ffn_g": [4, D],
    "ln_ffn_b": [4, D], "mlp_w1": [4, D, DFF], "mlp_w2": [4, DFF, D],
}
DSA_NCOLS = 2312


def dsa_colmap(dst):
    cm = []
    for gam in range(2):
        for r in range(4):
            tau = 4 * gam + r
            cm.append((64 * (8 * gam + r), 64, dst, 128 * tau))
            cm.append((64 * (8 * gam + 4 + r), 64, dst, 128 * tau + 64))
    cm.append((1024, 256, dst, 1024))
    for h in range(8):
        cm.append((1536 + 32 * h, 32, dst, 1536 + 128 * (h // 3) + 32 * (h % 3)))
    for rep in range(3):
        cm.append((1792, 32, dst, 1920 + 32 * rep))
    cm.append((1280, 256, dst, 2048))
    cm.append((1824, 8, dst, 2304))
    return cm


def build_program(nlayers=DEPTH):
    nc = bass.Bass("TRN2", target_bir_lowering=False)
    I = {k: nc.dram_tensor(k, v, F32, kind="ExternalInput").ap() for k, v in IN_SHAPES.items()}
    y = nc.dram_tensor("y", [T, D], F32, kind="ExternalOutput").ap()

    def scr(name, shape, dt):
        return nc.dram_tensor(name, shape, dt, kind="Internal").ap()
    XT = scr("XT", [D, T], BF16)
    X1 = scr("X1", [T, D], F32)
    X1T = scr("X1T", [D, T], BF16)
    XB = [scr("XB0", [T, D], F32), scr("XB1", [T, D], F32)]
    FM = scr("FM", [2048, T], BF16)
    TMV = scr("TMV", [T, 512], BF16)
    WIs = scr("WIs", [T, 8], F32)
    CAT = scr("CAT", [D, T], BF16)
    WIN = [scr("WIN%d" % l, [D, DSA_NCOLS], BF16) for l in range(nlayers)]
    WOUT = [scr("WOUT%d" % l, [D, D], BF16) for l in range(nlayers)]
    W1 = [scr("W1_%d" % l, [D, DFF], BF16) for l in range(nlayers)]
    W2 = [scr("W2_%d" % l, [DFF, D], BF16) for l in range(nlayers)]
    WG = [scr("WG%d" % j, [512, 512], BF16) for j in range(2)]

    with ExitStack() as es:
        P = Prog(nc, es)
        jobs = []
        for l in range(nlayers):
            j = l // 2
            if l % 2 == 0:
                jobs.append((I["sb_ssm_w_in"][j], [(0, 2048, WIN[l], 0)]))
                jobs.append((I["sb_ssm_w_out"][j], [(0, D, WOUT[l], 0)]))
                jobs.append((I["ssm_w_glu"][j], [(0, 512, WG[j], 0)]))
            else:
                jobs.append((I["dsa_w_in"][j], dsa_colmap(WIN[l])))
                jobs.append((I["dsa_w_out"][j], [(0, D, WOUT[l], 0)]))
            jobs.append((I["mlp_w1"][l], [(0, DFF, W1[l], 0)]))
            jobs.append((I["mlp_w2"][l], [(0, D, W2[l], 0)]))
        phase_cast(P, jobs)
        phase_transpose(P, I["x"], XT)
        xin = I["x"]
        for l in range(nlayers):
            j = l // 2
            last = (l == nlayers - 1)
            if l % 2 == 0:
                fm = [(128 * o, FM, 128 * o) for o in range(8)] + [(128 * o, FM, 1024 + 128 * (o - 12)) for o in range(12, 16)]
                phase_inproj(P, XT, WIN[l], 2048, fm, [(1024, 512, TMV, BF16)])
                phase_sb_attn(P, FM[0:1024, :], TMV, CAT)
                prm = (I["ssm_log_dt"][j], I["ssm_lam_re"][j], I["ssm_lam_im"][j], I["ssm_b_re"][j], I["ssm_b_im"][j],
                       I["ssm_c_re"][j], I["ssm_c_im"][j], I["ssm_d"][j], I["ssm_b_glu"][j])
                phase_ssm(P, FM[1024:1536, :], prm, WG[j], CAT)
            else:
                fm = [(128 * o, FM, 128 * o, 0.125) for o in range(8)] + [(128 * o, FM, 128 * o) for o in (8, 9, 12, 13, 14, 15)]
                phase_inproj(P, XT, WIN[l], DSA_NCOLS, fm, [(2048, 256, TMV[:, 0:256], BF16), (2304, 8, WIs, F32)])
                phase_dsa(P, FM, TMV[:, 0:256], WIs, CAT)
            phase_outproj_ln(P, CAT, WOUT[l], xin, I["ln_mix_g"][l:l + 1, :], I["ln_mix_b"][l:l + 1, :], X1, X1T)
            xout = y if last else XB[l % 2]
            phase_mlp(P, X1T, X1, W1[l], W2[l], I["ln_ffn_g"][l:l + 1, :], I["ln_ffn_b"][l:l + 1, :], xout,
                      None if last else XT)
            xin = xout
        P.barrier()
    return nc


_PROG = {}


def kernel(**inputs):
    if "nc" not in _PROG:
        _PROG["nc"] = build_program()
    nc = _PROG["nc"]
    x = np.ascontiguousarray(np.asarray(inputs["x"], dtype=np.float32))
    B = x.shape[0]
    shared = {k: np.ascontiguousarray(np.asarray(inputs[k], dtype=np.float32)) for k in IN_SHAPES if k != "x"}
    in_maps = []
    for b in range(B):
        m = dict(shared)
        m["x"] = x[b]
        in_maps.append(m)
    res = run_bass_kernel_spmd(nc, in_maps, core_ids=[4 + b for b in range(B)])
    return np.stack([np.asarray(r["y"], dtype=np.float32) for r in res.results], axis=0)
```
